# Optimizing a Trainium2 kernel written in Bass

```python
import math
import jax
import jax.numpy as jnp
from jax import lax
import numpy as np

D_MODEL = 1024
BATCH = 2
SEQ = 8192
DEPTH = 2

GRID_W = 64
CTX_LEN = 256
F32 = jnp.float32

N_EVEN = (DEPTH + 1) // 2
N_ODD = DEPTH // 2

RET_WIDTH = D_MODEL // 2
RET_HEADS = 4
RET_HEAD_DIM = RET_WIDTH // RET_HEADS
RET_CHUNK = 128
ROPE_BASE = 10000.0

HY_WIDTH = D_MODEL // 2
HY_ORDER = 2
HY_SHORT = 3
HY_EMB = 33
HY_FFN = 64
HY_SHORT_DECAY_PCT = 0.3
HY_LONG_DECAY_PCT = 1.5
HY_TARGET = 1e-2

AB_IN = 4 * RET_WIDTH + (HY_ORDER + 1) * HY_WIDTH
AB_OUT = RET_WIDTH + HY_WIDTH

GLA_HEADS = 4
GLA_KEY = D_MODEL // 2
GLA_VAL = D_MODEL
GLA_DK = GLA_KEY // GLA_HEADS
GLA_DV = GLA_VAL // GLA_HEADS
GLA_RANK = 16
GLA_TAU = 16.0
GLA_CHUNK = 64
GLA_SPLITS = [GLA_KEY, 2 * GLA_KEY, 2 * GLA_KEY + GLA_VAL, 2 * GLA_KEY + 2 * GLA_VAL,
              2 * GLA_KEY + 2 * GLA_VAL + GLA_RANK]
GLA_IN = 2 * GLA_KEY + 2 * GLA_VAL + 2 * GLA_RANK

N_EXPERTS = 16
N_GROUPS = 4
EXPERTS_PER_GROUP = N_EXPERTS // N_GROUPS
TOP_K = 2
EXPERT_HIDDEN = D_MODEL // 2

DN_ALPHA = (2.0 * DEPTH) ** 0.25
DN_BETA = (8.0 * DEPTH) ** -0.25
LN_EPS = 1e-5

kernel_name = 'hybrid_retention_hyena_gla_moe_dit'


def layer_norm(x, g, b):
    xf = x.astype(F32)
    mu = jnp.mean(xf, axis=-1, keepdims=True)
    var = jnp.mean(jnp.square(xf - mu), axis=-1, keepdims=True)
    return ((xf - mu) * lax.rsqrt(var + LN_EPS) * g.astype(F32) + b.astype(F32)).astype(x.dtype)


def head_layernorm(o):
    of = o.astype(F32)
    mu = jnp.mean(of, axis=-1, keepdims=True)
    var = jnp.mean(jnp.square(of - mu), axis=-1, keepdims=True)
    return ((of - mu) * lax.rsqrt(var + LN_EPS)).astype(o.dtype)


def head_rmsnorm(o):
    of = o.astype(F32)
    return (of * lax.rsqrt(jnp.mean(jnp.square(of), axis=-1, keepdims=True) + LN_EPS)).astype(o.dtype)


def ada(cond, w, b):
    return jnp.split(jax.nn.silu(cond) @ w + b, 6, axis=-1)


def post_norm(x, gate, y, g, b):
    return layer_norm(DN_ALPHA * x + gate * y, g, b)


def to_heads(t, n):
    bsz, length, width = t.shape
    return t.reshape(bsz, length, n, width // n).transpose(0, 2, 1, 3)


def from_heads(t):
    bsz, n, length, d = t.shape
    return t.transpose(0, 2, 1, 3).reshape(bsz, length, n * d)


def rev(t):
    return jnp.flip(t, axis=2)


def to_chunks(t, size):
    bsz, n, length, d = t.shape
    return jnp.moveaxis(t.reshape(bsz, n, length // size, size, d), 2, 0)


def from_chunks(t):
    nc, bsz, n, size, d = t.shape
    return jnp.moveaxis(t, 0, 2).reshape(bsz, n, nc * size, d)


def axial_rope(t, rows):
    hd = t.shape[-1]
    quarter = hd // 4
    inv = ROPE_BASE ** (-jnp.arange(quarter, dtype=F32) / quarter)
    r = jnp.repeat(jnp.arange(rows, dtype=F32), GRID_W)
    col = jnp.tile(jnp.arange(GRID_W, dtype=F32), rows)
    ang = jnp.concatenate([r[:, None] * inv, col[:, None] * inv], axis=-1)
    cos, sin = jnp.cos(ang), jnp.sin(ang)
    t1, t2 = jnp.split(t.astype(F32), 2, axis=-1)
    return jnp.concatenate([t1 * cos - t2 * sin, t1 * sin + t2 * cos], axis=-1).astype(t.dtype)


def retention_scan(q, k, v, log_gamma, s0):
    C = RET_CHUNK
    lg = log_gamma.astype(F32)[:, None]
    idx = jnp.arange(C, dtype=F32)
    diff = idx[:, None] - idx[None, :]
    intra = jnp.where(diff >= 0, jnp.exp(jnp.maximum(diff, 0.0)[None] * lg[:, :, None]), 0.0)
    q_decay = jnp.exp((idx + 1.0) * lg)[..., None]
    k_decay = jnp.exp((C - 1.0 - idx) * lg)[..., None]
    c_decay = jnp.exp(C * lg)[..., None]

    def step(s, inp):
        qc, kc, vc = inp
        scores = jnp.einsum('bhid,bhjd->bhij', qc, kc) * intra
        o = jnp.einsum('bhij,bhje->bhie', scores, vc) + jnp.einsum('bhid,bhde->bhie', qc * q_decay, s)
        s = c_decay * s + jnp.einsum('bhjd,bhje->bhde', kc * k_decay, vc)
        return s, o

    s_fin, o = lax.scan(step, s0, (to_chunks(q, C), to_chunks(k, C), to_chunks(v, C)))
    return from_chunks(o).astype(v.dtype), s_fin


def gla_scan(q, k, v, log_a, s0):
    C = GLA_CHUNK
    mask = jnp.tril(jnp.ones((C, C), dtype=bool))

    def step(s, inp):
        qc, kc, vc, ac = inp
        b = jnp.cumsum(ac, axis=2)
        qd = qc * jnp.exp(b)
        kd = kc * jnp.exp(-b)
        scores = jnp.where(mask, jnp.einsum('bhid,bhjd->bhij', qd, kd), 0.0)
        o = jnp.einsum('bhij,bhje->bhie', scores, vc) + jnp.einsum('bhid,bhde->bhie', qd, s)
        b_end = b[:, :, -1:, :]
        s = jnp.exp(b_end)[:, :, 0, :, None] * s + jnp.einsum('bhjd,bhje->bhde', kc * jnp.exp(b_end - b), vc)
        return s, o

    s_fin, o = lax.scan(step, s0, tuple(to_chunks(t, C) for t in (q, k, v, log_a)))
    return from_chunks(o).astype(v.dtype), s_fin


def final_state(k, v, log_a):
    la = jnp.broadcast_to(log_a.astype(F32), k.shape)
    cum = jnp.cumsum(la, axis=2)
    return jnp.einsum('bhld,bhle->bhde', k * jnp.exp(cum[:, :, -1:, :] - cum), v)


def retention_bidir(q, k, v, lg_f, lg_b, s0_f, s0_b):
    o_f, s_f = retention_scan(q, k, v, lg_f, s0_f)
    o_b, s_b = retention_scan(rev(q), rev(k), rev(v), lg_b, s0_b)
    return o_f + rev(o_b), s_f, s_b


def gla_bidir(q, k, v, la_f, la_b, s0_f, s0_b):
    o_f, s_f = gla_scan(q, k, v, la_f, s0_f)
    o_b, s_b = gla_scan(rev(q), rev(k), rev(v), rev(la_b), s0_b)
    return o_f + rev(o_b), s_f, s_b


def hyena_filters(length, w1, b1, w2, b2, w3, b3, w4, freq):
    t = jnp.linspace(0.0, 1.0, length, dtype=F32)[:, None]
    bands = (HY_EMB - 1) // 2
    w = 2.0 * math.pi * jnp.arange(length, dtype=F32)[:, None] / length
    f = jnp.linspace(1e-4, bands - 1, bands, dtype=F32)[None, :]
    feats = jnp.concatenate([t, jnp.cos(f * w), -jnp.sin(f * w)], axis=-1)
    a = jnp.sin(freq[0] * (feats @ w1 + b1))
    a = jnp.sin(freq[1] * (a @ w2 + b2))
    a = jnp.sin(freq[2] * (a @ w3 + b3))
    filt = (a @ w4).astype(F32).reshape(length, HY_ORDER, 2, HY_WIDTH)
    max_decay = math.log(HY_TARGET) / HY_SHORT_DECAY_PCT
    min_decay = math.log(HY_TARGET) / HY_LONG_DECAY_PCT
    deltas = jnp.abs(jnp.linspace(min_decay, max_decay, HY_WIDTH, dtype=F32))
    filt = filt * jnp.exp(-t * deltas)[:, None, None, :]
    return filt * lax.rsqrt(jnp.sum(filt * filt, axis=0, keepdims=True))


def bidir_long_conv(u, h_f, h_b):
    L = u.shape[1]
    taps = jnp.concatenate([h_f[:1] + h_b[:1], h_f[1:], jnp.zeros_like(h_f[:1]), jnp.flip(h_b[1:], axis=0)], axis=0)
    U = jnp.fft.rfft(u.astype(F32), n=2 * L, axis=1)
    H = jnp.fft.rfft(taps, n=2 * L, axis=0)
    return jnp.fft.irfft(U * H[None], n=2 * L, axis=1)[:, :L].astype(u.dtype)


def centred_depthwise_conv(u, w, b):
    pad = HY_SHORT // 2
    y = lax.conv_general_dilated(u, w[:, None, :].astype(u.dtype), window_strides=(1,), padding=((pad, pad),),
                                 dimension_numbers=('NWC', 'WIO', 'NWC'), feature_group_count=u.shape[-1])
    return y + b


def hyena(u, conv_w, conv_b, filt, skip):
    u = centred_depthwise_conv(u, conv_w, conv_b)
    x1, x2, v = jnp.split(u, 3, axis=-1)
    z = v
    for n, gate in enumerate((x1, x2)):
        z = gate * (bidir_long_conv(z, filt[:, n, 0], filt[:, n, 1]) + skip[n] * z)
    return z


def ret_project(z):
    q, k, v, g = jnp.split(z[..., :4 * RET_WIDTH], 4, axis=-1)
    return (to_heads(q, RET_HEADS), to_heads(k, RET_HEADS) * RET_HEAD_DIM ** -0.5,
            to_heads(v, RET_HEADS), g)


def ret_hyena_mixer(h, hc, rows, w_in, lg_f, lg_b, conv_w, conv_b, filt_w, skip, w_out, need_ctx_out):
    z = h @ w_in
    zc = hc @ w_in
    q, k, v, g = ret_project(z)
    qc, kc, vc, gc = ret_project(zc)
    q, k = axial_rope(q, rows), axial_rope(k, rows)
    if need_ctx_out:
        zero = jnp.zeros(kc.shape[:2] + (RET_HEAD_DIM, RET_HEAD_DIM), F32)
        oc, s_f, s_b = retention_bidir(qc, kc, vc, lg_f, lg_b, zero, zero)
    else:
        s_f = final_state(kc, vc, lg_f[:, None, None])
        s_b = final_state(rev(kc), rev(vc), lg_b[:, None, None])
    o, _, _ = retention_bidir(q, k, v, lg_f, lg_b, s_f, s_b)
    y_ret = from_heads(head_layernorm(o)) * jax.nn.silu(g)
    y_hy = hyena(z[..., 4 * RET_WIDTH:], conv_w, conv_b, hyena_filters(h.shape[1], *filt_w), skip)
    y = jnp.concatenate([y_ret, y_hy], axis=-1) @ w_out
    if not need_ctx_out:
        return y, None
    yc_ret = from_heads(head_layernorm(oc)) * jax.nn.silu(gc)
    yc_hy = hyena(zc[..., 4 * RET_WIDTH:], conv_w, conv_b, hyena_filters(hc.shape[1], *filt_w), skip)
    yc = jnp.concatenate([yc_ret, yc_hy], axis=-1) @ w_out
    return y, yc


def gla_project(hh, w_in, gw_f, gb_f, gw_b, gb_b):
    q, k, v, g, lr_f, lr_b = jnp.split(hh @ w_in, GLA_SPLITS, axis=-1)
    la_f = jax.nn.log_sigmoid((lr_f @ gw_f + gb_f).astype(F32)) / GLA_TAU
    la_b = jax.nn.log_sigmoid((lr_b @ gw_b + gb_b).astype(F32)) / GLA_TAU
    return (to_heads(q, GLA_HEADS) * GLA_DK ** -0.5, to_heads(k, GLA_HEADS), to_heads(v, GLA_HEADS), g,
            to_heads(la_f, GLA_HEADS), to_heads(la_b, GLA_HEADS))


def gla_mixer(h, hc, w_in, gw_f, gb_f, gw_b, gb_b, w_out, need_ctx_out):
    q, k, v, g, la_f, la_b = gla_project(h, w_in, gw_f, gb_f, gw_b, gb_b)
    qc, kc, vc, gc, lac_f, lac_b = gla_project(hc, w_in, gw_f, gb_f, gw_b, gb_b)
    if need_ctx_out:
        zero = jnp.zeros(kc.shape[:2] + (GLA_DK, GLA_DV), F32)
        oc, s_f, s_b = gla_bidir(qc, kc, vc, lac_f, lac_b, zero, zero)
    else:
        s_f = final_state(kc, vc, lac_f)
        s_b = final_state(rev(kc), rev(vc), rev(lac_b))
    o, _, _ = gla_bidir(q, k, v, la_f, la_b, s_f, s_b)
    y = (from_heads(head_rmsnorm(o)) * jax.nn.silu(g)) @ w_out
    if not need_ctx_out:
        return y, None
    yc = (from_heads(head_rmsnorm(oc)) * jax.nn.silu(gc)) @ w_out
    return y, yc


def moe(h, router_w, router_bias, w_gate, w_up, w_down):
    T = h.shape[0]
    s = jax.nn.sigmoid((h @ router_w).astype(F32))
    sel = s + router_bias.astype(F32)
    grp_score = jnp.sum(lax.top_k(sel.reshape(T, N_GROUPS, EXPERTS_PER_GROUP), TOP_K)[0], axis=-1)
    best = jnp.argmax(grp_score, axis=-1)
    in_group = (jnp.arange(N_EXPERTS) // EXPERTS_PER_GROUP)[None, :] == best[:, None]
    _, idx = lax.top_k(jnp.where(in_group, sel, -jnp.inf), TOP_K)
    w = jnp.take_along_axis(s, idx, axis=-1)
    w = w / jnp.sum(w, axis=-1, keepdims=True)
    combine = jnp.sum(jax.nn.one_hot(idx, N_EXPERTS, dtype=F32) * w[..., None], axis=1)
    y = jnp.zeros((T, h.shape[1]), F32)
    for e in range(N_EXPERTS):
        he = jax.nn.silu(h @ w_gate[e]) * (h @ w_up[e])
        y = y + combine[:, e:e + 1] * (he @ w_down[e])
    return y.astype(h.dtype)


def setup_inputs(seed: int = 0) -> dict:
    key = jax.random.key(seed)
    ks = iter(jax.random.split(key, 40))

    def nrm(shape, scale):
        return jax.random.normal(next(ks), shape, F32) * scale

    base_decay = jnp.log(1.0 - 2.0 ** (-5.0 - jnp.arange(RET_HEADS, dtype=F32)))
    return {
        'x': nrm((BATCH, SEQ, D_MODEL), 1.0),
        'c': nrm((BATCH, D_MODEL), 1.0),
        'ctx': nrm((BATCH, CTX_LEN, D_MODEL), 1.0),
        'c_ctx': nrm((D_MODEL,), 1.0),
        'mod_w': nrm((DEPTH, D_MODEL, 6 * D_MODEL), 0.5 * D_MODEL ** -0.5),
        'mod_b': nrm((DEPTH, 6 * D_MODEL), 0.02),
        'ln_g': 1.0 + nrm((DEPTH, 2, D_MODEL), 0.02),
        'ln_b': nrm((DEPTH, 2, D_MODEL), 0.02),
        'ab_w_in': nrm((N_EVEN, D_MODEL, AB_IN), D_MODEL ** -0.5),
        'ret_log_decay_f': base_decay * (1.0 + nrm((N_EVEN, RET_HEADS), 0.05)),
        'ret_log_decay_b': base_decay * (1.0 + nrm((N_EVEN, RET_HEADS), 0.05)),
        'hy_conv_w': nrm((N_EVEN, HY_SHORT, (HY_ORDER + 1) * HY_WIDTH), HY_SHORT ** -0.5),
        'hy_conv_b': nrm((N_EVEN, (HY_ORDER + 1) * HY_WIDTH), 0.02),
        'hy_w1': nrm((N_EVEN, HY_EMB, HY_FFN), HY_EMB ** -0.5),
        'hy_b1': nrm((N_EVEN, HY_FFN), 0.1),
        'hy_w2': nrm((N_EVEN, HY_FFN, HY_FFN), HY_FFN ** -0.5),
        'hy_b2': nrm((N_EVEN, HY_FFN), 0.1),
        'hy_w3': nrm((N_EVEN, HY_FFN, HY_FFN), HY_FFN ** -0.5),
        'hy_b3': nrm((N_EVEN, HY_FFN), 0.1),
        'hy_w4': nrm((N_EVEN, HY_FFN, HY_ORDER * 2 * HY_WIDTH), HY_FFN ** -0.5),
        'hy_freq': 1.0 + nrm((N_EVEN, 3, HY_FFN), 0.1),
        'hy_skip': nrm((N_EVEN, HY_ORDER, HY_WIDTH), 0.5),
        'ab_w_out': nrm((N_EVEN, AB_OUT, D_MODEL), DN_BETA * AB_OUT ** -0.5),
        'gla_w_in': nrm((N_ODD, D_MODEL, GLA_IN), D_MODEL ** -0.5),
        'gla_gate_w_f': nrm((N_ODD, GLA_RANK, GLA_KEY), GLA_RANK ** -0.5),
        'gla_gate_b_f': nrm((N_ODD, GLA_KEY), 0.1),
        'gla_gate_w_b': nrm((N_ODD, GLA_RANK, GLA_KEY), GLA_RANK ** -0.5),
        'gla_gate_b_b': nrm((N_ODD, GLA_KEY), 0.1),
        'gla_w_out': nrm((N_ODD, GLA_VAL, D_MODEL), DN_BETA * GLA_VAL ** -0.5),
        'router_w': nrm((D_MODEL, N_EXPERTS), D_MODEL ** -0.5),
        'router_bias': nrm((N_EXPERTS,), 0.01),
        'exp_w_gate': nrm((DEPTH, N_EXPERTS, D_MODEL, EXPERT_HIDDEN), D_MODEL ** -0.5),
        'exp_w_up': nrm((DEPTH, N_EXPERTS, D_MODEL, EXPERT_HIDDEN), D_MODEL ** -0.5),
        'exp_w_down': nrm((DEPTH, N_EXPERTS, EXPERT_HIDDEN, D_MODEL), DN_BETA * EXPERT_HIDDEN ** -0.5),
    }


def reference(x, c, ctx, c_ctx, mod_w, mod_b, ln_g, ln_b, ab_w_in, ret_log_decay_f, ret_log_decay_b,
              hy_conv_w, hy_conv_b, hy_w1, hy_b1, hy_w2, hy_b2, hy_w3, hy_b3, hy_w4, hy_freq, hy_skip,
              ab_w_out, gla_w_in, gla_gate_w_f, gla_gate_b_f, gla_gate_w_b, gla_gate_b_b, gla_w_out,
              router_w, router_bias, exp_w_gate, exp_w_up, exp_w_down):
    B, L, D = x.shape
    rows = L // GRID_W
    for layer in range(DEPTH):
        last = layer == DEPTH - 1
        i = layer // 2
        sh1, sc1, g1, sh2, sc2, g2 = [t[:, None, :] for t in ada(c, mod_w[layer], mod_b[layer])]
        csh1, csc1, cg1, csh2, csc2, cg2 = ada(c_ctx, mod_w[layer], mod_b[layer])
        h = x * (1.0 + sc1) + sh1
        hc = ctx * (1.0 + csc1) + csh1
        if layer % 2 == 0:
            filt_w = (hy_w1[i], hy_b1[i], hy_w2[i], hy_b2[i], hy_w3[i], hy_b3[i], hy_w4[i], hy_freq[i])
            y, yc = ret_hyena_mixer(h, hc, rows, ab_w_in[i], ret_log_decay_f[i], ret_log_decay_b[i],
                                    hy_conv_w[i], hy_conv_b[i], filt_w, hy_skip[i], ab_w_out[i], not last)
        else:
            y, yc = gla_mixer(h, hc, gla_w_in[i], gla_gate_w_f[i], gla_gate_b_f[i], gla_gate_w_b[i],
                              gla_gate_b_b[i], gla_w_out[i], not last)
        x = post_norm(x, g1, y, ln_g[layer, 0], ln_b[layer, 0])
        h = (x * (1.0 + sc2) + sh2).reshape(B * L, D)
        if last:
            out = moe(h, router_w, router_bias, exp_w_gate[layer], exp_w_up[layer], exp_w_down[layer])
            x = post_norm(x, g2, out.reshape(B, L, D), ln_g[layer, 1], ln_b[layer, 1])
        else:
            ctx = post_norm(ctx, cg1, yc, ln_g[layer, 0], ln_b[layer, 0])
            n_ctx = ctx.shape[1]
            hc = (ctx * (1.0 + csc2) + csh2).reshape(B * n_ctx, D)
            out = moe(jnp.concatenate([h, hc], axis=0), router_w, router_bias,
                      exp_w_gate[layer], exp_w_up[layer], exp_w_down[layer])
            x = post_norm(x, g2, out[:B * L].reshape(B, L, D), ln_g[layer, 1], ln_b[layer, 1])
            ctx = post_norm(ctx, cg2, out[B * L:].reshape(B, n_ctx, D), ln_g[layer, 1], ln_b[layer, 1])
    return x
```

```python
import math
from contextlib import ExitStack
import numpy as np
import concourse.bass as bass
import concourse.mybir as mybir
from concourse.bass_utils import run_bass_kernel_spmd

F32 = mybir.dt.float32
BF16 = mybir.dt.bfloat16
I32 = mybir.dt.int32
U8 = mybir.dt.uint8
AF = mybir.ActivationFunctionType
ALU = mybir.AluOpType
AX = mybir.AxisListType


class Buf:
    __slots__ = ("name", "t", "last_w", "readers", "dsem", "dcnt", "w_is_dma", "is_dram")

    def __init__(self, name, t):
        self.name = name
        self.t = t
        self.last_w = []
        self.readers = []
        self.dsem = None
        self.dcnt = 0
        self.w_is_dma = False
        self.is_dram = False

    def __getitem__(self, idx):
        return self.t[idx]


class Prog:
    def __init__(self):
        self.nc = bass.Bass("TRN2", target_bir_lowering=False)
        self.es = ExitStack()
        nc = self.nc
        self.eng = {"pe": nc.tensor, "dve": nc.vector, "act": nc.scalar, "pool": nc.gpsimd, "sp": nc.sync}
        self.sem = {}
        self.cnt = {}
        self.seen = {}
        for e in self.eng:
            self.sem[e] = self.es.enter_context(nc.semaphore("s_" + e))
            self.cnt[e] = 0
            self.seen[e] = {}
        self.nbuf = 0
        self.dma_owners = []
        self.tmp = None
        self.ninst = 0

    def sbuf(self, name, shape, dt=F32):
        es = self.tmp if self.tmp is not None else self.es
        t = es.enter_context(self.nc.sbuf_tensor(name, list(shape), dt))
        return Buf(name, t)

    def begin_tmp(self):
        self.tmp = ExitStack()

    def end_tmp(self):
        self.barrier()
        self.tmp.close()
        self.tmp = None

    def psum(self, name, shape, dt=F32):
        t = self.es.enter_context(self.nc.psum_tensor(name, list(shape), dt))
        return Buf(name, t)

    def dram(self, name, shape, dt=F32, kind="Internal"):
        t = self.nc.dram_tensor(name, list(shape), dt, kind=kind)
        b = Buf(name, t.ap())
        b.is_dram = True
        return b

    def view(self, name, ap):
        return Buf(name, ap)

    def _deps(self, reads, writes, group=False):
        deps = []
        for r in reads:
            deps.extend(r.last_w)
        for w in writes:
            if not (group and w.w_is_dma):
                deps.extend(w.last_w)
            deps.extend(w.readers)
        return deps

    def _wait(self, e, deps):
        seen = self.seen[e]
        need = {}
        for (s, v) in deps:
            k = id(s)
            if seen.get(k, (None, 0))[1] >= v:
                continue
            if k not in need or need[k][1] < v:
                need[k] = (s, v)
        for k, (s, v) in need.items():
            self.eng[e].wait_ge(s, v)
            seen[k] = (s, v)

    def _commit(self, tok, reads, writes, is_dma=False, group=False):
        for w in writes:
            if group and w.w_is_dma and is_dma:
                w.last_w = [t for t in w.last_w if t[0] is not tok[0]] + [tok]
            else:
                w.last_w = [tok]
            w.readers = []
            w.w_is_dma = is_dma
        for r in reads:
            if r not in writes:
                r.readers.append(tok)

    def op(self, e, fn, reads=(), writes=()):
        self._wait(e, self._deps(reads, writes))
        ins = fn(self.eng[e])
        self.cnt[e] += 1
        ins.then_inc(self.sem[e], 1)
        tok = (self.sem[e], self.cnt[e])
        self._commit(tok, reads, writes)
        self.ninst += 1
        return tok

    def mm(self, out, pairs, reads, transpose=False):
        e = "pe"
        self._wait(e, self._deps(reads, [out]))
        n = len(pairs)
        ins = None
        for i, (o, l, r) in enumerate(pairs):
            ins = self.nc.tensor.matmul(o, l, r, start=(i == 0), stop=(i == n - 1))
        self.cnt[e] += 1
        ins.then_inc(self.sem[e], 1)
        tok = (self.sem[e], self.cnt[e])
        self._commit(tok, reads, [out])
        self.ninst += n
        return tok

    def mms(self, out, triples, reads):
        e = "pe"
        self._wait(e, self._deps(reads, [out]))
        ins = None
        for (o, l, r, st, sp) in triples:
            ins = self.nc.tensor.matmul(o, l, r, start=st, stop=sp)
        self.cnt[e] += 1
        ins.then_inc(self.sem[e], 1)
        tok = (self.sem[e], self.cnt[e])
        self._commit(tok, reads, [out])
        self.ninst += len(triples)
        return tok

    def dma(self, q, out_ap, in_ap, reads, writes, group=False, **kw):
        w = writes[0]
        if w.is_dram:
            w = reads[0]
            assert not w.is_dram
        if w.dsem is None:
            w.dsem = self.es.enter_context(self.nc.semaphore("d_" + w.name))
            self.dma_owners.append(w)
        self._wait(q, self._deps(reads, writes, group=group))
        ins = self.eng[q].dma_start(out=out_ap, in_=in_ap, **kw)
        w.dcnt += 16
        ins.then_inc(w.dsem, 16)
        tok = (w.dsem, w.dcnt)
        self._commit(tok, reads, writes, is_dma=True, group=group)
        self.ninst += 1
        return tok

    def finish(self, bufs):
        deps = []
        for b in bufs:
            deps.extend(b.last_w)
        self._wait("sp", deps)
        return self.nc

    def barrier(self):
        toks = [(self.sem[e], self.cnt[e]) for e in self.eng if self.cnt[e] > 0]
        toks += [(b.dsem, b.dcnt) for b in self.dma_owners if b.dcnt > 0]
        for e in self.eng:
            self._wait(e, toks)

    def close(self):
        self.es.close()


D = 1024
NE = 16
EH = 512
DN_ALPHA = (2.0 * 2) ** 0.25
LN_EPS = 1e-5


def ln_tile(p, u, n, g_b, b_b, st, mv, eps_t, affine_eng="pool"):
    p.op("dve", lambda e: e.bn_stats(out=st[:n, 0, :], in_=u[:n, 0:512]), [u], [st])
    p.op("dve", lambda e: e.bn_stats(out=st[:n, 1, :], in_=u[:n, 512:1024]), [u, st], [st])
    p.op("dve", lambda e: e.bn_aggr(out=mv[:n, 0:2], in_=st[:n].rearrange("p a b -> p (a b)")), [st], [mv])
    p.op("act", lambda e: e.activation(out=mv[:n, 2:3], in_=mv[:n, 1:2], func=AF.Sqrt, bias=eps_t[:n, 0:1], scale=1.0),
         [mv, eps_t], [mv])
    p.op("dve", lambda e: e.reciprocal(out=mv[:n, 3:4], in_=mv[:n, 2:3]), [mv], [mv])
    p.op("dve", lambda e: e.tensor_scalar(out=u[:n], in0=u[:n], scalar1=mv[:n, 0:1], scalar2=mv[:n, 3:4],
                                          op0=ALU.subtract, op1=ALU.mult), [u, mv], [u])
    p.op(affine_eng, lambda e: e.tensor_tensor(out=u[:n], in0=u[:n], in1=g_b[:n], op=ALU.mult), [u, g_b], [u])
    p.op(affine_eng, lambda e: e.tensor_tensor(out=u[:n], in0=u[:n], in1=b_b[:n], op=ALU.add), [u, b_b], [u])


def build_ffn(has_ctx, kin=1024):
    p = Prog()
    nc = p.nc
    NT = 2048 + (64 if has_ctx else 0)
    tiles = [(i * 128, 128, 0) for i in range(16)] + ([(2048, 64, 1)] if has_ctx else [])
    groups = [tiles[0:6], tiles[6:12], tiles[12:]]
    GT = 768
    KC = kin // 128
    nmod = 2 if has_ctx else 1
    yT = p.dram("yT", [kin, NT], F32, kind="ExternalInput")
    xres = p.dram("xres", [NT, D], F32, kind="ExternalInput")
    cvec = p.dram("cvec", [D, 2], F32, kind="ExternalInput")
    modw = p.dram("modw", [D, 4096], F32, kind="ExternalInput")
    modb = p.dram("modb", [1, 4096], F32, kind="ExternalInput")
    modbT = p.dram("modbT", [128, 16], F32, kind="ExternalInput")
    lng = p.dram("lng", [2, D], F32, kind="ExternalInput")
    lnb = p.dram("lnb", [2, D], F32, kind="ExternalInput")
    wout = p.dram("wout", [kin, D], F32, kind="ExternalInput")
    rw = p.dram("rw", [D, NE], F32, kind="ExternalInput")
    rbias = p.dram("rbias", [1, NE], F32, kind="ExternalInput")
    wg = p.dram("wg", [NE, D, EH], F32, kind="ExternalInput")
    wu = p.dram("wu", [NE, D, EH], F32, kind="ExternalInput")
    wd = p.dram("wd", [NE, EH, D], F32, kind="ExternalInput")
    ident = p.dram("ident", [128, 128], F32, kind="ExternalInput")
    out = p.dram("out", [NT, D], F32, kind="ExternalOutput")

    ident_s = p.sbuf("ident_s", [128, 128], F32)
    eps_t = p.sbuf("eps_t", [128, 1], F32)
    g1t = [p.sbuf(f"g1t{v}", [128, D], F32) for v in range(nmod)]
    g2t = [p.sbuf(f"g2t{v}", [128, D], F32) for v in range(nmod)]
    fmv = [p.sbuf(f"fmv{v}", [128, 16], F32) for v in range(nmod)]
    lngt = [p.sbuf(f"lng{j}", [128, D], F32) for j in range(2)]
    lnbt = [p.sbuf(f"lnb{j}", [128, D], F32) for j in range(2)]
    rbt = p.sbuf("rbt", [128, NE], F32)
    wout_s = p.sbuf("wout_s", [128, KC, D], BF16)
    rw_s = p.sbuf("rw_s", [128, 8, NE], F32)
    banks = [p.psum(f"bank{j}", [128, 512], F32) for j in range(8)]

    p.dma("sp", ident_s[:], ident[:], [ident], [ident_s])
    p.op("dve", lambda e: e.memset(eps_t[:], LN_EPS), [], [eps_t])
    for j in range(2):
        p.dma("sp", lngt[j][:], lng[j:j + 1, :].partition_broadcast(128), [lng], [lngt[j]])
        p.dma("sp", lnbt[j][:], lnb[j:j + 1, :].partition_broadcast(128), [lnb], [lnbt[j]])
    p.dma("sp", rbt[:], rbias[0:1, :].partition_broadcast(128), [rbias], [rbt])
    p.dma("pool", wout_s[:], wout[:].rearrange("(k p) n -> p k n", p=128), [wout], [wout_s])
    p.dma("sp", rw_s[:], rw[:].rearrange("(k p) n -> p k n", p=128), [rw], [rw_s])

    p.begin_tmp()
    ones_s = p.sbuf("ones_s", [128, 128], F32)
    cv = p.sbuf("cv", [128, 8, 2], F32)
    cbs = [p.sbuf(f"cb{j}", [128, 8, 128], F32) for j in range(nmod)]
    mw = [p.sbuf(f"mw{j}", [128, 8, 512], F32) for j in range(2)]
    mbb = [p.sbuf(f"mbb{j}", [128, 512], F32) for j in range(2)]
    mbT = p.sbuf("mbT", [128, 16], F32)
    p.op("dve", lambda e: e.memset(ones_s[:], 1.0), [], [ones_s])
    p.dma("sp", cv[:], cvec[:].rearrange("(k p) c -> p k c", p=128), [cvec], [cv])
    p.dma("sp", mbT[:], modbT[:], [modbT], [mbT])
    p.op("act", lambda e: e.activation(out=cv[:], in_=cv[:], func=AF.Silu), [cv], [cv])
    for v in range(nmod):
        for k in range(8):
            p.op("act", lambda e: e.activation(out=cbs[v][:, k, :], in_=ones_s[:], func=AF.Copy, scale=cv[:, k, v:v + 1]),
                 [ones_s, cv], [cbs[v]])
    for blk in range(8):
        sect, half = blk // 2, blk % 2
        m = mw[blk % 2]
        p.dma("sp", m[:], modw[:, blk * 512:(blk + 1) * 512].rearrange("(k p) n -> p k n", p=128), [modw], [m])
        if sect in (0, 3):
            bb = mbb[half]
            p.dma("act", bb[:], modb[0:1, blk * 512:(blk + 1) * 512].partition_broadcast(128), [modb], [bb])
            for v in range(nmod):
                ps = banks[v]
                p.mm(ps, [(ps[:], cbs[v][:, k, :], m[:, k, :]) for k in range(8)], [cbs[v], m])
                dst = (g1t if sect == 0 else g2t)[v]
                p.op("dve", lambda e: e.tensor_tensor(out=dst[:, half * 512:(half + 1) * 512], in0=ps[:], in1=bb[:], op=ALU.add),
                     [ps, bb], [dst])
        else:
            ps = banks[2 + blk % 2]
            for jj in range(4):
                ps_ap = ps[:, jj * 2:jj * 2 + 2]
                if jj == 0:
                    p._wait("pe", p._deps([m, cv], [ps]))
                ins = None
                for k in range(8):
                    ins = nc.tensor.matmul(ps_ap, m[:, k, jj * 128:(jj + 1) * 128], cv[:, k, 0:2], start=(k == 0), stop=(k == 7))
            p.cnt["pe"] += 1
            ins.then_inc(p.sem["pe"], 1)
            p._commit((p.sem["pe"], p.cnt["pe"]), [m, cv], [ps])
            c0 = (0 if sect == 1 else 8) + half * 4
            for v in range(nmod):
                p.op("dve", lambda e: e.tensor_tensor(out=fmv[v][:, c0:c0 + 4],
                                                      in0=ps[:, 0:8].rearrange("p (j v) -> p j v", v=2)[:, :, v],
                                                      in1=mbT[:, c0:c0 + 4], op=ALU.add), [ps, mbT], [fmv[v]])
    for v in range(nmod):
        p.op("dve", lambda e: e.tensor_scalar(out=fmv[v][:, 8:16], in0=fmv[v][:, 8:16], scalar1=1.0, scalar2=None, op0=ALU.add),
             [fmv[v]], [fmv[v]])
    p.end_tmp()

    ybuf = p.sbuf("ybuf", [128, KC * GT], BF16)
    yT_s = ybuf[:].rearrange("p (k t) -> p k t", k=KC)
    heT = ybuf[:, 0:4 * GT].rearrange("p (k t) -> p k t", k=4)
    h2T = p.sbuf("h2T", [128, 8, GT], BF16)
    x1 = [p.sbuf(f"x1_{i}", [128, D], F32) for i in range(6)]
    acc = [p.sbuf(f"acc_{i}", [128, D], F32) for i in range(6)]
    comb = [p.sbuf(f"comb_{i}", [128, NE], F32) for i in range(6)]
    xin = [p.sbuf(f"xin{j}", [128, D], F32) for j in range(2)]
    h2T32 = p.sbuf("h2T32", [128, 8, 128], F32)
    stats = p.sbuf("stats", [128, 2, 6], F32)
    mv = p.sbuf("mv", [128, 4], F32)
    rt = [p.sbuf(f"rt{j}", [128, NE], F32) for j in range(6)]
    rs = p.sbuf("rs", [128, 8], F32)
    wg_s = [p.sbuf(f"wg_s{j}", [128, 8, EH], BF16) for j in range(2)]
    wu_s = [p.sbuf(f"wu_s{j}", [128, 8, EH], BF16) for j in range(2)]
    wd_s = [p.sbuf(f"wd_s{j}", [128, 4, D], BF16) for j in range(2)]

    wcount = 0
    for gi, grp in enumerate(groups):
        g0 = grp[0][0]
        gtok = sum(n for (_, n, _) in grp)
        p.dma("pool", yT_s[:, :, 0:gtok], yT[:, g0:g0 + gtok].rearrange("(k p) n -> p k n", p=128), [yT], [ybuf])
        for ti, (t0, n, v) in enumerate(grp):
            lo = t0 - g0
            xi = xin[ti % 2]
            u = x1[ti]
            p.dma("sp", xi[:n], xres[t0:t0 + n, :], [xres], [xi])
            for half in range(2):
                ps = banks[half]
                p.mm(ps, [(ps[:n], yT_s[:, k, lo:lo + n], wout_s[:, k, half * 512:(half + 1) * 512]) for k in range(KC)],
                     [ybuf, wout_s])
            for half in range(2):
                sl = slice(half * 512, (half + 1) * 512)
                p.op("dve", lambda e: e.tensor_tensor(out=u[:n, sl], in0=banks[half][:n], in1=g1t[v][:n, sl], op=ALU.mult),
                     [banks[half], g1t[v]], [u])
            p.op("dve", lambda e: e.scalar_tensor_tensor(out=u[:n], in0=xi[:n], scalar=DN_ALPHA, in1=u[:n],
                                                         op0=ALU.mult, op1=ALU.add), [xi, u], [u])
            ln_tile(p, u, n, lngt[0], lnbt[0], stats, mv, eps_t)
            for half in range(2):
                ps = banks[2 + half]

                def tr(e):
                    ins = None
                    for kk in range(4):
                        k = half * 4 + kk
                        ins = e.transpose(ps[:, kk * 128:kk * 128 + n], u[:n, k * 128:(k + 1) * 128], ident_s[:n, :n])
                    return ins
                p.op("pe", tr, [u, ident_s], [ps])
                for kk in range(4):
                    k = half * 4 + kk
                    p.op("act", lambda e: e.activation(out=h2T32[:, k, 0:n], in_=ps[:, kk * 128:kk * 128 + n], func=AF.Identity,
                                                       scale=fmv[v][:, 8 + k:9 + k], bias=fmv[v][:, k:k + 1]), [ps, fmv[v]], [h2T32])
            p.op("pool", lambda e: e.tensor_copy(out=h2T[:, :, lo:lo + n], in_=h2T32[:, :, 0:n]), [h2T32], [h2T])
            ps = banks[4]
            p.mm(ps, [(ps[:n, 0:NE], h2T32[:, k, 0:n], rw_s[:, k, :]) for k in range(8)], [h2T32, rw_s])
            s_, sel, t1, t2, t3, t4 = rt
            p.op("act", lambda e: e.activation(out=s_[:n], in_=ps[:n, 0:NE], func=AF.Sigmoid), [ps], [s_])
            p.op("dve", lambda e: e.tensor_tensor(out=sel[:n], in0=s_[:n], in1=rbt[:n], op=ALU.add), [s_, rbt], [sel])
            sel3 = sel[:n].rearrange("p (g e) -> p g e", g=4)
            p.op("dve", lambda e: e.tensor_reduce(out=rs[:n, 0:4], in_=sel3, axis=AX.X, op=ALU.max), [sel], [rs])
            p.op("dve", lambda e: e.tensor_tensor(out=t1[:n].rearrange("p (g e) -> p g e", g=4), in0=sel3,
                                                  in1=rs[:n, 0:4].unsqueeze(2).to_broadcast([n, 4, 4]), op=ALU.is_equal), [sel, rs], [t1])
            p.op("dve", lambda e: e.scalar_tensor_tensor(out=t2[:n], in0=t1[:n], scalar=-8.0, in1=sel[:n], op0=ALU.mult, op1=ALU.add),
                 [t1, sel], [t2])
            p.op("dve", lambda e: e.tensor_reduce(out=rs[:n, 4:8], in_=t2[:n].rearrange("p (g e) -> p g e", g=4), axis=AX.X, op=ALU.max),
                 [t2, rs], [rs])
            p.op("dve", lambda e: e.tensor_tensor(out=rs[:n, 0:4], in0=rs[:n, 0:4], in1=rs[:n, 4:8], op=ALU.add), [rs], [rs])
            p.op("dve", lambda e: e.tensor_reduce(out=rs[:n, 4:5], in_=rs[:n, 0:4], axis=AX.X, op=ALU.max), [rs], [rs])
            p.op("dve", lambda e: e.tensor_scalar(out=rs[:n, 0:4], in0=rs[:n, 0:4], scalar1=rs[:n, 4:5], scalar2=None, op0=ALU.is_equal),
                 [rs], [rs])
            p.op("dve", lambda e: e.scalar_tensor_tensor(out=t1[:n].rearrange("p (g e) -> p g e", g=4), in0=sel3, scalar=4.0,
                                                         in1=rs[:n, 0:4].unsqueeze(2).to_broadcast([n, 4, 4]), op0=ALU.add, op1=ALU.mult),
                 [sel, rs], [t1])
            p.op("dve", lambda e: e.tensor_reduce(out=rs[:n, 5:6], in_=t1[:n], axis=AX.X, op=ALU.max), [t1, rs], [rs])
            p.op("dve", lambda e: e.tensor_scalar(out=t2[:n], in0=t1[:n], scalar1=rs[:n, 5:6], scalar2=None, op0=ALU.is_equal), [t1, rs], [t2])
            p.op("dve", lambda e: e.scalar_tensor_tensor(out=t3[:n], in0=t2[:n], scalar=-16.0, in1=t1[:n], op0=ALU.mult, op1=ALU.add),
                 [t2, t1], [t3])
            p.op("dve", lambda e: e.tensor_reduce(out=rs[:n, 6:7], in_=t3[:n], axis=AX.X, op=ALU.max), [t3, rs], [rs])
            p.op("dve", lambda e: e.tensor_scalar(out=t4[:n], in0=t3[:n], scalar1=rs[:n, 6:7], scalar2=None, op0=ALU.is_equal), [t3, rs], [t4])
            p.op("dve", lambda e: e.tensor_tensor(out=t2[:n], in0=t2[:n], in1=t4[:n], op=ALU.add), [t2, t4], [t2])
            p.op("dve", lambda e: e.tensor_tensor(out=t2[:n], in0=t2[:n], in1=s_[:n], op=ALU.mult), [t2, s_], [t2])
            p.op("dve", lambda e: e.tensor_reduce(out=rs[:n, 7:8], in_=t2[:n], axis=AX.X, op=ALU.add), [t2, rs], [rs])
            p.op("dve", lambda e: e.reciprocal(out=rs[:n, 7:8], in_=rs[:n, 7:8]), [rs], [rs])
            cb = comb[ti]
            p.op("dve", lambda e: e.tensor_scalar(out=cb[:n], in0=t2[:n], scalar1=rs[:n, 7:8], scalar2=None, op0=ALU.mult), [t2, rs], [cb])
        blocks = []
        o = 0
        while o < gtok:
            bn = min(512, gtok - o)
            blocks.append((o, bn))
            o += bn
        for ex in range(NE):
            wsel = wcount % 2
            wcount += 1
            g_s, u_s, d_s = wg_s[wsel], wu_s[wsel], wd_s[wsel]
            p.dma("pool", g_s[:], wg[ex].rearrange("(k p) n -> p k n", p=128), [wg], [g_s])
            p.dma("pool", u_s[:], wu[ex].rearrange("(k p) n -> p k n", p=128), [wu], [u_s])
            p.dma("pool", d_s[:], wd[ex].rearrange("(k p) n -> p k n", p=128), [wd], [d_s])
            cnt = 0
            for (b0, bn) in blocks:
                for j in range(4):
                    pg = banks[cnt % 2]
                    pu = banks[2 + cnt % 2]
                    sl_ = xin[cnt % 2]
                    cnt += 1
                    p.mm(pg, [(pg[:, 0:bn], g_s[:, k, j * 128:(j + 1) * 128], h2T[:, k, b0:b0 + bn]) for k in range(8)], [g_s, h2T])
                    p.mm(pu, [(pu[:, 0:bn], u_s[:, k, j * 128:(j + 1) * 128], h2T[:, k, b0:b0 + bn]) for k in range(8)], [u_s, h2T])
                    p.op("act", lambda e: e.activation(out=sl_[:, 0:bn], in_=pg[:, 0:bn], func=AF.Silu), [pg], [sl_])
                    p.op("dve", lambda e: e.tensor_tensor(out=heT[:, j, b0:b0 + bn], in0=pu[:, 0:bn], in1=sl_[:, 0:bn], op=ALU.mult),
                         [pu, sl_], [ybuf])
            for ti, (t0, n, v) in enumerate(grp):
                lo = t0 - g0
                for half in range(2):
                    ps = banks[4 + (ti * 2 + half) % 4]
                    p.mm(ps, [(ps[:n], heT[:, j, lo:lo + n], d_s[:, j, half * 512:(half + 1) * 512]) for j in range(4)], [ybuf, d_s])
                    sl = slice(half * 512, (half + 1) * 512)
                    a = acc[ti]
                    if ex == 0:
                        p.op("dve", lambda e: e.tensor_scalar(out=a[:n, sl], in0=ps[:n], scalar1=comb[ti][:n, ex:ex + 1], scalar2=None,
                                                              op0=ALU.mult), [ps, comb[ti]], [a])
                    else:
                        p.op("dve", lambda e: e.scalar_tensor_tensor(out=a[:n, sl], in0=ps[:n], scalar=comb[ti][:n, ex:ex + 1], in1=a[:n, sl],
                                                                     op0=ALU.mult, op1=ALU.add), [ps, comb[ti], a], [a])
        for ti, (t0, n, v) in enumerate(grp):
            a = acc[ti]
            p.op("pool", lambda e: e.tensor_tensor(out=a[:n], in0=a[:n], in1=g2t[v][:n], op=ALU.mult), [a, g2t[v]], [a])
            p.op("dve", lambda e: e.scalar_tensor_tensor(out=a[:n], in0=x1[ti][:n], scalar=DN_ALPHA, in1=a[:n], op0=ALU.mult, op1=ALU.add),
                 [x1[ti], a], [a])
            ln_tile(p, a, n, lngt[1], lnbt[1], stats, mv, eps_t)
            p.dma("sp", out[t0:t0 + n, :], a[:n], [a], [out], group=True)
    p.finish([out])
    p.close()
    return nc


D = 1024
LN_EPS = 1e-5


def compute_fm_mod(p, modw, modbT, cvec, banks, name="fm"):
    nc = p.nc
    fmv = [p.sbuf(f"{name}{v}", [128, 16], F32) for v in range(2)]
    p.begin_tmp()
    cv = p.sbuf(name + "_cv", [128, 8, 2], F32)
    mw = [p.sbuf(f"{name}_mw{j}", [128, 8, 512], F32) for j in range(2)]
    mbT = p.sbuf(name + "_mbT", [128, 16], F32)
    p.dma("sp", cv[:], cvec[:].rearrange("(k p) c -> p k c", p=128), [cvec], [cv])
    p.dma("sp", mbT[:], modbT[:], [modbT], [mbT])
    p.op("act", lambda e: e.activation(out=cv[:], in_=cv[:], func=AF.Silu), [cv], [cv])
    for blk in range(4):
        sect, half = blk // 2, blk % 2
        m = mw[blk % 2]
        p.dma("sp", m[:], modw[:, blk * 512:(blk + 1) * 512].rearrange("(k p) n -> p k n", p=128), [modw], [m])
        ps = banks[blk % 2]

        def mmf(e):
            ins = None
            for jj in range(4):
                for k in range(8):
                    ins = e.matmul(ps[:, jj * 2:jj * 2 + 2], m[:, k, jj * 128:(jj + 1) * 128], cv[:, k, 0:2], start=(k == 0), stop=(k == 7))
            return ins
        p.op("pe", mmf, [m, cv], [ps])
        c0 = sect * 8 + half * 4
        for v in range(2):
            p.op("dve", lambda e: e.tensor_tensor(out=fmv[v][:, c0:c0 + 4], in0=ps[:, 0:8].rearrange("p (j v) -> p j v", v=2)[:, :, v],
                                                  in1=mbT[:, c0:c0 + 4], op=ALU.add), [ps, mbT], [fmv[v]])
    for v in range(2):
        p.op("dve", lambda e: e.tensor_scalar(out=fmv[v][:, 8:16], in0=fmv[v][:, 8:16], scalar1=1.0, scalar2=None, op0=ALU.add),
             [fmv[v]], [fmv[v]])
    p.end_tmp()
    return fmv


def build_gla():
    p = Prog()
    nc = p.nc
    L, LC = 8192, 256
    DK, DV = 128, 256
    NW = 800
    xT = p.dram("xT", [D, L], F32, kind="ExternalInput")
    cxT = p.dram("cxT", [D, LC], F32, kind="ExternalInput")
    cvec = p.dram("cvec", [D, 2], F32, kind="ExternalInput")
    modw = p.dram("modw", [D, 2048], F32, kind="ExternalInput")
    modbT = p.dram("modbT", [128, 16], F32, kind="ExternalInput")
    w = p.dram("w", [D, NW], F32, kind="ExternalInput")
    gwe = p.dram("gwe", [2, 17, DK], F32, kind="ExternalInput")
    cst = p.dram("cst", [4, 128, 128], F32, kind="ExternalInput")
    msk = p.dram("msk", [2, 128, 128], F32, kind="ExternalInput")
    feat = p.dram("feat", [L, DV], F32, kind="ExternalOutput")
    of_scr = p.dram("of_scr", [L, DV], F32, kind="Internal")

    banks = [p.psum(f"bank{j}", [128, 512], F32) for j in range(8)]
    fmv = compute_fm_mod(p, modw, modbT, cvec, banks)
    bA, bB, bC, bD, bE, bF, bGH, bI = banks

    w_s = p.sbuf("w_s", [128, 8, NW], BF16)
    p.dma("pool", w_s[:], w[:].rearrange("(k p) n -> p k n", p=128), [w], [w_s])
    gw_s = p.sbuf("gw_s", [17, 2, DK], F32)
    p.dma("sp", gw_s[:], gwe[:].rearrange("d r n -> r d n"), [gwe], [gw_s])
    cst_s = p.sbuf("cst_s", [128, 4, 128], F32)
    p.dma("sp", cst_s[:], cst[:].rearrange("c p n -> p c n"), [cst], [cst_s])
    msk_s = p.sbuf("msk_s", [128, 2, 128], F32)
    p.dma("sp", msk_s[:], msk[:].rearrange("c p n -> p c n"), [msk], [msk_s])
    eps_t = p.sbuf("eps_t", [128, 1], F32)
    p.op("dve", lambda e: e.memset(eps_t[:], LN_EPS), [], [eps_t])

    xb = [p.sbuf(f"xb{j}", [128, 8, 512], F32) for j in range(2)]
    hT = [p.sbuf(f"hT{j}", [128, 8, 512], BF16) for j in range(2)]
    lrx = [p.sbuf(f"lrx{j}", [17, 512], F32) for j in range(2)]
    for j in range(2):
        p.op("dve", lambda e: e.memset(lrx[j][:], 1.0), [], [lrx[j]])
    NB = 2
    t_abs = [p.sbuf(f"t_abs{j}", [128, 128], F32) for j in range(NB)]
    t_la = [p.sbuf(f"t_la{j}", [128, 128], F32) for j in range(NB)]
    t_eb = [p.sbuf(f"t_eb{j}", [128, 128], F32) for j in range(NB)]
    t_enb = [p.sbuf(f"t_enb{j}", [128, 128], F32) for j in range(NB)]
    t_es = [p.sbuf(f"t_es{j}", [128, 128], F32) for j in range(NB)]
    t_qd = [p.sbuf(f"t_qd{j}", [128, 128], BF16) for j in range(NB)]
    t_qd2 = [p.sbuf(f"t_qd2{j}", [128, 2, 128], BF16) for j in range(NB)]
    t_kd = [p.sbuf(f"t_kd{j}", [128, 128], BF16) for j in range(NB)]
    t_kk = [p.sbuf(f"t_kk{j}", [128, 128], BF16) for j in range(NB)]
    t_v = [p.sbuf(f"t_v{j}", [128, DV], BF16) for j in range(NB)]
    t_sT = [p.sbuf(f"t_sT{j}", [128, 128], BF16) for j in range(NB)]
    t_o = [p.sbuf(f"t_o{j}", [128, DV], F32) for j in range(NB)]
    t_of = [p.sbuf(f"t_of{j}", [128, DV], F32) for j in range(NB)]
    t_sg = [p.sbuf(f"t_sg{j}", [128, DV], F32) for j in range(NB)]
    t_sq = [p.sbuf(f"t_sq{j}", [128, DV], F32) for j in range(NB)]
    t_r = [p.sbuf(f"t_r{j}", [128, 4], F32) for j in range(NB)]
    for j in range(NB):
        p.op("pool", lambda e: e.memset(t_qd2[j][:], 0.0), [], [t_qd2[j]])
    S = p.sbuf("S", [128, DV], F32)
    S_bf = [p.sbuf(f"S_bf{j}", [128, DV], BF16) for j in range(2)]

    tcount = 0
    bcount = 0
    import os
    NPASS = int(os.environ.get("GLA_PASSES", "2"))
    NBLK = int(os.environ.get("GLA_BLOCKS", "17"))
    STAGE = float(os.environ.get("GLA_STAGE", "9"))
    for dd in range(NPASS):
        p.op("dve", lambda e: e.memset(S[:], 0.0), [], [S])
        p.op("pool", lambda e: e.memset(S_bf[0][:], 0.0), [], [S_bf[0]])
        scur = 0
        lat = [(xT, i * 512, 512, False) for i in range(16)]
        blocks = [(cxT, 0, 256, True)] + lat if dd == 0 else [(cxT, 0, 256, True)] + lat[::-1]
        for (src, t0, nt, is_ctx) in blocks[:NBLK]:
            v = 1 if is_ctx else 0
            bsel = bcount % 2
            bcount += 1
            x_ = xb[bsel]
            h_ = hT[bsel]
            lr_ = lrx[bsel]
            if STAGE < 1:
                continue
            p.dma("sp", x_[:, :, 0:nt], src[:, t0:t0 + nt].rearrange("(k p) n -> p k n", p=128), [src], [x_])
            for k in range(8):
                p.op("act", lambda e: e.activation(out=h_[:, k, 0:nt], in_=x_[:, k, 0:nt], func=AF.Identity,
                                                   scale=fmv[v][:, 8 + k:9 + k], bias=fmv[v][:, k:k + 1]), [x_, fmv[v]], [h_])
            p.mm(bA, [(bA[:, 0:nt], w_s[:, k, 0:128], h_[:, k, 0:nt]) for k in range(8)], [w_s, h_])
            p.mm(bB, [(bB[:, 0:nt], w_s[:, k, 128:256], h_[:, k, 0:nt]) for k in range(8)], [w_s, h_])
            lc = 768 + 16 * dd
            p.mm(bC, [(bC[0:16, 0:nt], w_s[:, k, lc:lc + 16], h_[:, k, 0:nt]) for k in range(8)], [w_s, h_])
            p.op("act", lambda e: e.activation(out=lr_[0:16, 0:nt], in_=bC[0:16, 0:nt], func=AF.Identity), [bC], [lr_])
            ntile = nt // 128
            if STAGE < 2:
                continue
            order = list(range(ntile)) if dd == 0 else list(range(ntile))[::-1]
            for ti in order:
                ts = slice(ti * 128, (ti + 1) * 128)
                tg = t0 + ti * 128
                sel = tcount % NB
                tcount += 1
                a_, la_, eb_, enb_, es_ = t_abs[sel], t_la[sel], t_eb[sel], t_enb[sel], t_es[sel]
                qd_, qd2_, kd_, kk_, v_, sT_ = t_qd[sel], t_qd2[sel], t_kd[sel], t_kk[sel], t_v[sel], t_sT[sel]
                p.mm(bD, [(bD[:, 0:384], h_[:, k, ts], w_s[:, k, 128:512]) for k in range(8)], [h_, w_s])
                need_out = not is_ctx
                if need_out and dd == 1:
                    p.mm(bE, [(bE[:, 0:256], h_[:, k, ts], w_s[:, k, 512:768]) for k in range(8)], [h_, w_s])
                if STAGE < 2.2:
                    continue
                zF = p_view(p, bF)
                p.mm(bF, [(bF[:, 0:128], lr_[0:17, ts], gw_s[0:17, dd, :])], [lr_, gw_s])
                p.op("act", lambda e: e.activation(out=la_[:], in_=bF[:, 0:128], func=AF.Identity), [bF], [la_])
                if STAGE < 2.4:
                    continue
                p.op("dve", lambda e: e.scalar_tensor_tensor(out=a_[:], in0=la_[:], scalar=-1.0, in1=la_[:], op0=ALU.mult, op1=ALU.min), [la_], [a_])
                if STAGE < 2.6:
                    continue
                p.op("act", lambda e: e.activation(out=a_[:], in_=a_[:], func=AF.Exp), [a_], [a_])
                if STAGE < 2.8:
                    continue
                p.op("act", lambda e: e.activation(out=a_[:], in_=a_[:], func=AF.Ln, bias=1.0), [a_], [a_])
                if STAGE < 2.95:
                    continue
                p.op("dve", lambda e: e.tensor_scalar(out=la_[:], in0=la_[:], scalar1=0.0, scalar2=None, op0=ALU.min), [la_], [la_])
                p.op("dve", lambda e: e.tensor_tensor(out=la_[:], in0=la_[:], in1=a_[:], op=ALU.subtract), [la_, a_], [la_])
                if STAGE < 3:
                    continue
                def mm2(e):
                    e.matmul(bF[:, 128:256], la_[:], cst_s[:, dd, :], start=True, stop=True)
                    return e.matmul(bF[:, 256:384], cst_s[:, 2 + dd, :], la_[:], start=True, stop=True)
                p.op("pe", mm2, [la_, cst_s], [bF])
                if STAGE < 3.2:
                    continue
                p.op("act", lambda e: e.activation(out=eb_[:], in_=bF[:, 128:256], func=AF.Exp), [bF], [eb_])
                p.op("act", lambda e: e.activation(out=enb_[:], in_=bF[:, 128:256], func=AF.Exp, scale=-1.0), [bF], [enb_])
                p.op("act", lambda e: e.activation(out=es_[:], in_=bF[:, 256:384], func=AF.Exp), [bF], [es_])
                if need_out:
                    p.op("dve", lambda e: e.scalar_tensor_tensor(out=qd_[:], in0=bA[:, ts], scalar=DK ** -0.5, in1=eb_[:], op0=ALU.mult, op1=ALU.mult),
                         [bA, eb_], [qd_])
                    p.op("pool", lambda e: e.tensor_copy(out=qd2_[:, 0, 0:64], in_=qd_[:, 0:64]), [qd_], [qd2_])
                    p.op("pool", lambda e: e.tensor_copy(out=qd2_[:, 1, 64:128], in_=qd_[:, 64:128]), [qd_, qd2_], [qd2_])
                    p.op("dve", lambda e: e.tensor_tensor(out=kd_[:], in0=bB[:, ts], in1=enb_[:], op=ALU.mult), [bB, enb_], [kd_])
                if STAGE < 3.4:
                    continue
                p.op("dve", lambda e: e.tensor_tensor(out=kk_[:], in0=bD[:, 0:128], in1=es_[:], op=ALU.mult), [bD, es_], [kk_])
                if STAGE < 3.6:
                    continue
                p.op("act", lambda e: e.activation(out=v_[:], in_=bD[:, 128:384], func=AF.Identity), [bD], [v_])
                if need_out:
                    p.mm(bGH, [(bGH[:, 0:128], kd_[:], qd_[:])], [kd_, qd_])
                    p.op("dve", lambda e: e.tensor_tensor(out=sT_[:], in0=bGH[:, 0:128], in1=msk_s[:, dd, :], op=ALU.mult), [bGH, msk_s], [sT_])
                if STAGE < 4:
                    continue
                halves = [0, 1] if dd == 0 else [1, 0]
                dcol = {0: (63 if dd == 0 else 0), 1: (127 if dd == 0 else 64)}
                s_in = S_bf[scur]
                s_mid = S_bf[1 - scur]
                hf, hs = halves
                if need_out:
                    def o1(e):
                        e.matmul(bI[:, 0:DV], sT_[:], v_[:], start=True, stop=False)
                        return e.matmul(bI[:, 0:DV], qd2_[:, hf, :], s_in[:], start=False, stop=False)
                    p.op("pe", o1, [sT_, v_, qd2_, s_in], [bI])
                p.mm(bGH, [(bGH[:, 256:512], kk_[hf * 64:(hf + 1) * 64, :], v_[hf * 64:(hf + 1) * 64, :])], [kk_, v_])
                p.op("dve", lambda e: e.scalar_tensor_tensor(out=S[:], in0=S[:], scalar=eb_[:, dcol[hf]:dcol[hf] + 1], in1=bGH[:, 256:512],
                                                             op0=ALU.mult, op1=ALU.add), [S, eb_, bGH], [S])
                p.op("act", lambda e: e.activation(out=s_mid[:], in_=S[:], func=AF.Identity), [S], [s_mid])
                if need_out:
                    p.op("pe", lambda e: e.matmul(bI[:, 0:DV], qd2_[:, hs, :], s_mid[:], start=False, stop=True), [qd2_, s_mid, bI], [bI])
                p.mm(bGH, [(bGH[:, 256:512], kk_[hs * 64:(hs + 1) * 64, :], v_[hs * 64:(hs + 1) * 64, :])], [kk_, v_])
                p.op("dve", lambda e: e.scalar_tensor_tensor(out=S[:], in0=S[:], scalar=eb_[:, dcol[hs]:dcol[hs] + 1], in1=bGH[:, 256:512],
                                                             op0=ALU.mult, op1=ALU.add), [S, eb_, bGH], [S])
                p.op("act", lambda e: e.activation(out=s_in[:], in_=S[:], func=AF.Identity), [S], [s_in])
                if need_out:
                    o_ = t_o[sel]
                    if dd == 0:
                        p.op("act", lambda e: e.activation(out=o_[:], in_=bI[:, 0:DV], func=AF.Identity), [bI], [o_])
                        p.dma("sp", of_scr[tg:tg + 128, :], o_[:], [o_], [of_scr], group=True)
                    else:
                        of_, sg_, sq_, r_ = t_of[sel], t_sg[sel], t_sq[sel], t_r[sel]
                        p.dma("sp", of_[:], of_scr[tg:tg + 128, :], [of_scr], [of_])
                        p.op("dve", lambda e: e.tensor_tensor(out=o_[:], in0=bI[:, 0:DV], in1=of_[:], op=ALU.add), [bI, of_], [o_])
                        p.op("act", lambda e: e.activation(out=sq_[:], in_=o_[:], func=AF.Square, accum_out=r_[:, 0:1]), [o_], [sq_, r_])
                        p.op("act", lambda e: e.activation(out=r_[:, 1:2], in_=r_[:, 0:1], func=AF.Sqrt, bias=eps_t[:, 0:1], scale=1.0 / DV),
                             [r_, eps_t], [r_])
                        p.op("dve", lambda e: e.reciprocal(out=r_[:, 2:3], in_=r_[:, 1:2]), [r_], [r_])
                        p.op("act", lambda e: e.activation(out=sg_[:], in_=bE[:, 0:256], func=AF.Silu), [bE], [sg_])
                        p.op("dve", lambda e: e.scalar_tensor_tensor(out=o_[:], in0=o_[:], scalar=r_[:, 2:3], in1=sg_[:], op0=ALU.mult, op1=ALU.mult),
                             [o_, r_, sg_], [o_])
                        p.dma("sp", feat[tg:tg + 128, :], o_[:], [o_], [feat], group=True)
    p.finish([feat])
    p.close()
    return nc


def p_view(p, b):
    return b


D = 1024
LN_EPS = 1e-5


def build_ret():
    p = Prog()
    nc = p.nc
    L, LC = 8192, 256
    HD = 128
    xT = p.dram("xT", [D, L], F32, kind="ExternalInput")
    cxT = p.dram("cxT", [D, LC], F32, kind="ExternalInput")
    cvec = p.dram("cvec", [D, 2], F32, kind="ExternalInput")
    modw = p.dram("modw", [D, 2048], F32, kind="ExternalInput")
    modbT = p.dram("modbT", [128, 16], F32, kind="ExternalInput")
    w = p.dram("w", [D, 768], F32, kind="ExternalInput")
    lgs = p.dram("lgs", [1, 2], F32, kind="ExternalInput")
    ecst = p.dram("ecst", [2, 128, 258], F32, kind="ExternalInput")
    msk = p.dram("msk", [2, 128, 128], F32, kind="ExternalInput")
    rope = p.dram("rope", [L, 128], F32, kind="ExternalInput")
    ropeT = p.dram("ropeT", [2, 128, L], F32, kind="ExternalInput")
    identb = p.dram("identb", [128, 128], F32, kind="ExternalInput")
    yret = p.dram("yret", [L + LC, HD], F32, kind="ExternalOutput")
    of_scr = p.dram("of_scr", [L + LC, HD], F32, kind="Internal")

    banks = [p.psum(f"bank{j}", [128, 512], F32) for j in range(8)]
    fmv = compute_fm_mod(p, modw, modbT, cvec, banks)
    bA, bQ, bQs, bK, bKs, bS, bV, bO = banks

    w_s = p.sbuf("w_s", [128, 8, 768], BF16)
    p.dma("pool", w_s[:], w[:].rearrange("(k p) n -> p k n", p=128), [w], [w_s])
    for (c0, c1) in ((128, 256), (640, 768)):
        p.op("dve", lambda e: e.tensor_scalar(out=w_s[:, :, c0:c1], in0=w_s[:, :, c0:c1], scalar1=128 ** -0.5, scalar2=None, op0=ALU.mult),
             [w_s], [w_s])
    ident_s = p.sbuf("ident_s", [128, 128], F32)
    p.dma("sp", ident_s[:], identb[:], [identb], [ident_s])
    msk_s = p.sbuf("msk_s", [128, 2, 128], F32)
    p.dma("sp", msk_s[:], msk[:].rearrange("c p n -> p c n"), [msk], [msk_s])
    lg_s = p.sbuf("lg_s", [128, 2], F32)
    p.dma("sp", lg_s[:], lgs[0:1, :].partition_broadcast(128), [lgs], [lg_s])
    ec_s = p.sbuf("ec_s", [128, 2, 258], F32)
    p.dma("sp", ec_s[:], ecst[:].rearrange("c p n -> p c n"), [ecst], [ec_s])
    tb_m = [p.sbuf(f"tb_m{d}", [128, 128], F32) for d in range(2)]
    tb_q = [p.sbuf(f"tb_q{d}", [128, 128], F32) for d in range(2)]
    tb_k = [p.sbuf(f"tb_k{d}", [128, 1], F32) for d in range(2)]
    tb_c = [p.sbuf(f"tb_c{d}", [128, 1], F32) for d in range(2)]
    for dd in range(2):
        p.op("act", lambda e: e.activation(out=tb_m[dd][:], in_=ec_s[:, dd, 0:128], func=AF.Exp, scale=lg_s[:, dd:dd + 1]), [ec_s, lg_s], [tb_m[dd]])
        p.op("act", lambda e: e.activation(out=tb_q[dd][:], in_=ec_s[:, dd, 128:256], func=AF.Exp, scale=lg_s[:, dd:dd + 1]), [ec_s, lg_s], [tb_q[dd]])
        p.op("act", lambda e: e.activation(out=tb_k[dd][:], in_=ec_s[:, dd, 256:257], func=AF.Exp, scale=lg_s[:, dd:dd + 1]), [ec_s, lg_s], [tb_k[dd]])
        p.op("act", lambda e: e.activation(out=tb_c[dd][:], in_=ec_s[:, dd, 257:258], func=AF.Exp, scale=lg_s[:, dd:dd + 1]), [ec_s, lg_s], [tb_c[dd]])
        p.op("dve", lambda e: e.tensor_tensor(out=tb_m[dd][:], in0=tb_m[dd][:], in1=msk_s[:, dd, :], op=ALU.mult), [tb_m[dd], msk_s], [tb_m[dd]])
    ksc_t = p.sbuf("ksc_t", [128, 1], F32)
    p.op("dve", lambda e: e.memset(ksc_t[:], 128 ** -0.5), [], [ksc_t])
    eps_t = p.sbuf("eps_t", [128, 1], F32)
    p.op("dve", lambda e: e.memset(eps_t[:], LN_EPS), [], [eps_t])

    xb = [p.sbuf(f"xb{j}", [128, 8, 512], F32) for j in range(2)]
    hT = [p.sbuf(f"hT{j}", [128, 8, 512], BF16) for j in range(2)]
    rT = [p.sbuf(f"rT{j}", [128, 2, 512], F32) for j in range(2)]
    b_t1 = [p.sbuf(f"b_t1{j}", [128, 512], F32) for j in range(2)]
    b_t2 = [p.sbuf(f"b_t2{j}", [128, 512], F32) for j in range(2)]
    b_qr = [p.sbuf(f"b_qr{j}", [128, 512], BF16) for j in range(2)]
    b_qd = [p.sbuf(f"b_qd{j}", [128, 512], BF16) for j in range(2)]
    b_kT = [p.sbuf(f"b_kT{j}", [128, 512], BF16) for j in range(2)]
    NB = 2
    t_rp = [p.sbuf(f"t_rp{j}", [128, 128], F32) for j in range(NB)]
    t_A = [p.sbuf(f"t_A{j}", [128, 128], F32) for j in range(NB)]
    t_B = [p.sbuf(f"t_B{j}", [128, 128], F32) for j in range(NB)]
    t_kr = [p.sbuf(f"t_kr{j}", [128, 128], F32) for j in range(NB)]
    t_kk = [p.sbuf(f"t_kk{j}", [128, 128], BF16) for j in range(NB)]
    t_v = [p.sbuf(f"t_v{j}", [128, HD], BF16) for j in range(NB)]
    t_sT = [p.sbuf(f"t_sT{j}", [128, 128], BF16) for j in range(NB)]
    t_o = [p.sbuf(f"t_o{j}", [128, HD], F32) for j in range(NB)]
    t_of = [p.sbuf(f"t_of{j}", [128, HD], F32) for j in range(NB)]
    t_sg = [p.sbuf(f"t_sg{j}", [128, HD], F32) for j in range(NB)]
    t_st = [p.sbuf(f"t_st{j}", [128, 6], F32) for j in range(NB)]
    t_r = [p.sbuf(f"t_r{j}", [128, 4], F32) for j in range(NB)]
    S = p.sbuf("S", [128, HD], F32)
    S_bf = p.sbuf("S_bf", [128, HD], BF16)
    KSC = HD ** -0.5

    tcount = 0
    bcount = 0
    import os
    NPASS = int(os.environ.get("RET_PASSES", "2"))
    NBLK = int(os.environ.get("RET_BLOCKS", "17"))
    STAGE = float(os.environ.get("RET_STAGE", "9"))
    for dd in range(NPASS):
        p.op("dve", lambda e: e.memset(S[:], 0.0), [], [S])
        p.op("pool", lambda e: e.memset(S_bf[:], 0.0), [], [S_bf])
        lat = [(xT, i * 512, 512, False) for i in range(16)]
        blocks = [(cxT, 0, 256, True)] + (lat if dd == 0 else lat[::-1])
        for (src, t0, nt, is_ctx) in blocks[:NBLK]:
            v = 1 if is_ctx else 0
            if STAGE < 0.2:
                continue
            bsel = bcount % 2
            bcount += 1
            x_, h_ = xb[bsel], hT[bsel]
            p.dma("sp", x_[:, :, 0:nt], src[:, t0:t0 + nt].rearrange("(k p) n -> p k n", p=128), [src], [x_])
            for k in range(8):
                p.op("act", lambda e: e.activation(out=h_[:, k, 0:nt], in_=x_[:, k, 0:nt], func=AF.Identity,
                                                   scale=fmv[v][:, 8 + k:9 + k], bias=fmv[v][:, k:k + 1]), [x_, fmv[v]], [h_])
            qr_, qd_, kTb = b_qr[bsel], b_qd[bsel], b_kT[bsel]
            if STAGE < 0.3:
                continue
            ntile = nt // 128
            p.mm(bQ, [(bQ[:, 0:nt], w_s[:, k, 0:128], h_[:, k, 0:nt]) for k in range(8)], [w_s, h_])
            p.mm(bK, [(bK[:, 0:nt], w_s[:, k, 128:256], h_[:, k, 0:nt]) for k in range(8)], [w_s, h_])
            if STAGE < 0.4:
                continue
            if not is_ctx:
                r_ = rT[bsel]
                t1, t2 = b_t1[bsel], b_t2[bsel]
                p.dma("act", r_[:], ropeT[:, :, t0:t0 + 512].rearrange("c p n -> p c n"), [ropeT], [r_])
                p.mm(bQs, [(bQs[:, 0:nt], w_s[:, k, 512:640], h_[:, k, 0:nt]) for k in range(8)], [w_s, h_])
                p.mm(bKs, [(bKs[:, 0:nt], w_s[:, k, 640:768], h_[:, k, 0:nt]) for k in range(8)], [w_s, h_])
                p.op("dve", lambda e: e.tensor_tensor(out=t1[:], in0=bQ[:], in1=r_[:, 0, :], op=ALU.mult), [bQ, r_], [t1])
                p.op("dve", lambda e: e.tensor_tensor(out=t2[:], in0=bQs[:], in1=r_[:, 1, :], op=ALU.mult), [bQs, r_], [t2])
                p.op("pool", lambda e: e.tensor_tensor(out=t1[:], in0=t1[:], in1=t2[:], op=ALU.add), [t1, t2], [t1])
                p.op("act", lambda e: e.activation(out=qr_[:], in_=t1[:], func=AF.Identity), [t1], [qr_])
                for a in range(4):
                    p.op("dve", lambda e: e.tensor_tensor(out=qd_[:, a * 128:(a + 1) * 128], in0=t1[:, a * 128:(a + 1) * 128],
                                                          in1=tb_q[dd][:], op=ALU.mult), [t1, tb_q[dd]], [qd_])
                p.op("dve", lambda e: e.tensor_tensor(out=t2[:], in0=bK[:], in1=r_[:, 0, :], op=ALU.mult), [bK, r_, t2], [t2])
                p.op("dve", lambda e: e.tensor_tensor(out=t1[:], in0=bKs[:], in1=r_[:, 1, :], op=ALU.mult), [bKs, r_, t1], [t1])
                p.op("dve", lambda e: e.scalar_tensor_tensor(out=kTb[:], in0=t1[:], scalar=1.0, in1=t2[:], op0=ALU.mult, op1=ALU.add), [t1, t2], [kTb])
            else:
                p.op("dve", lambda e: e.tensor_copy(out=qr_[:, 0:nt], in_=bQ[:, 0:nt]), [bQ], [qr_])
                if STAGE < 0.45:
                    continue
                for a in range(ntile):
                    p.op("dve", lambda e: e.tensor_tensor(out=qd_[:, a * 128:(a + 1) * 128], in0=bQ[:, a * 128:(a + 1) * 128],
                                                          in1=tb_q[dd][:], op=ALU.mult), [bQ, tb_q[dd]], [qd_])
                if STAGE < 0.48:
                    continue
                p.op("act", lambda e: e.activation(out=kTb[:, 0:nt], in_=bK[:, 0:nt], func=AF.Identity), [bK], [kTb])
            order = list(range(ntile)) if dd == 0 else list(range(ntile))[::-1]
            if STAGE < 1:
                continue
            for ti in order:
                ts = slice(ti * 128, (ti + 1) * 128)
                tg = t0 + ti * 128
                orow = (L + tg) if is_ctx else tg
                sel = tcount % NB
                tcount += 1
                rp_, A_, B_, kr_, kk_, v_, sT_ = t_rp[sel], t_A[sel], t_B[sel], t_kr[sel], t_kk[sel], t_v[sel], t_sT[sel]
                ncol = 384 if dd == 1 else 256
                p.mm(bA, [(bA[:, 0:128], h_[:, k, ts], w_s[:, k, 128:256]) for k in range(8)], [h_, w_s])
                p.mm(bV, [(bV[:, 0:ncol - 128], h_[:, k, ts], w_s[:, k, 256:128 + ncol]) for k in range(8)], [h_, w_s])
                if not is_ctx:
                    p.dma("act", rp_[:], rope[tg:tg + 128, :], [rope], [rp_])
                    k2 = bA[:, 0:128].rearrange("p (a d) -> p a d", a=2)
                    p.op("dve", lambda e: e.tensor_tensor(out=A_[:].rearrange("p (a d) -> p a d", a=2), in0=k2,
                                                          in1=rp_[:, 0:64].unsqueeze(1).to_broadcast([128, 2, 64]), op=ALU.mult), [bA, rp_], [A_])
                    p.op("dve", lambda e: e.tensor_tensor(out=B_[:].rearrange("p (a d) -> p a d", a=2), in0=k2,
                                                          in1=rp_[:, 64:128].unsqueeze(1).to_broadcast([128, 2, 64]), op=ALU.mult), [bA, rp_], [B_])
                    p.op("pool", lambda e: e.tensor_tensor(out=kr_[:, 0:64], in0=A_[:, 0:64], in1=B_[:, 64:128], op=ALU.subtract), [A_, B_], [kr_])
                    p.op("pool", lambda e: e.tensor_tensor(out=kr_[:, 64:128], in0=B_[:, 0:64], in1=A_[:, 64:128], op=ALU.add), [A_, B_, kr_], [kr_])
                    p.op("dve", lambda e: e.tensor_scalar(out=kk_[:], in0=kr_[:], scalar1=tb_k[dd][:, 0:1], scalar2=None,
                                                          op0=ALU.mult), [kr_, tb_k[dd]], [kk_])
                else:
                    p.op("dve", lambda e: e.tensor_scalar(out=kk_[:], in0=bA[:, 0:128], scalar1=tb_k[dd][:, 0:1], scalar2=None,
                                                          op0=ALU.mult), [bA, tb_k[dd]], [kk_])
                if STAGE < 2:
                    continue
                p.op("act", lambda e: e.activation(out=v_[:], in_=bV[:, 0:128], func=AF.Identity), [bV], [v_])
                p.mm(bS, [(bS[:, 0:128], kTb[:, ts], qr_[:, ts])], [kTb, qr_])
                p.op("dve", lambda e: e.tensor_tensor(out=sT_[:], in0=bS[:, 0:128], in1=tb_m[dd][:], op=ALU.mult), [bS, tb_m[dd]], [sT_])
                def om(e):
                    e.matmul(bO[:, 0:HD], sT_[:], v_[:], start=True, stop=False)
                    return e.matmul(bO[:, 0:HD], qd_[:, ts], S_bf[:], start=False, stop=True)
                p.op("pe", om, [sT_, v_, qd_, S_bf], [bO])
                p.mm(bS, [(bS[:, 256:256 + HD], kk_[:], v_[:])], [kk_, v_])
                p.op("dve", lambda e: e.scalar_tensor_tensor(out=S[:], in0=S[:], scalar=tb_c[dd][:, 0:1], in1=bS[:, 256:256 + HD],
                                                             op0=ALU.mult, op1=ALU.add), [S, tb_c[dd], bS], [S])
                p.op("act", lambda e: e.activation(out=S_bf[:], in_=S[:], func=AF.Identity), [S], [S_bf])
                if STAGE < 3:
                    continue
                o_ = t_o[sel]
                if dd == 0:
                    p.op("act", lambda e: e.activation(out=o_[:], in_=bO[:, 0:HD], func=AF.Identity), [bO], [o_])
                    p.dma("sp", of_scr[orow:orow + 128, :], o_[:], [o_], [of_scr], group=True)
                else:
                    of_, sg_, st_, r4 = t_of[sel], t_sg[sel], t_st[sel], t_r[sel]
                    p.dma("sp", of_[:], of_scr[orow:orow + 128, :], [of_scr], [of_])
                    p.op("dve", lambda e: e.tensor_tensor(out=o_[:], in0=bO[:, 0:HD], in1=of_[:], op=ALU.add), [bO, of_], [o_])
                    p.op("dve", lambda e: e.bn_stats(out=st_[:], in_=o_[:]), [o_], [st_])
                    p.op("dve", lambda e: e.bn_aggr(out=r4[:, 0:2], in_=st_[:]), [st_], [r4])
                    p.op("act", lambda e: e.activation(out=r4[:, 2:3], in_=r4[:, 1:2], func=AF.Sqrt, bias=eps_t[:, 0:1], scale=1.0), [r4, eps_t], [r4])
                    p.op("dve", lambda e: e.reciprocal(out=r4[:, 3:4], in_=r4[:, 2:3]), [r4], [r4])
                    p.op("dve", lambda e: e.tensor_scalar(out=o_[:], in0=o_[:], scalar1=r4[:, 0:1], scalar2=r4[:, 3:4],
                                                          op0=ALU.subtract, op1=ALU.mult), [o_, r4], [o_])
                    p.op("act", lambda e: e.activation(out=sg_[:], in_=bV[:, 128:256], func=AF.Silu), [bV], [sg_])
                    p.op("dve", lambda e: e.tensor_tensor(out=o_[:], in0=o_[:], in1=sg_[:], op=ALU.mult), [o_, sg_], [o_])
                    p.dma("sp", yret[orow:orow + 128, :], o_[:], [o_], [yret], group=True)
    p.finish([yret])
    p.close()
    return nc

import math

D = 1024
MAGIC = 12582912.0
TWO_PI = 2.0 * math.pi


def build_hy():
    p = Prog()
    nc = p.nc
    L, LC = 8192, 256
    xT = p.dram("xT", [D, L], F32, kind="ExternalInput")
    cxT = p.dram("cxT", [D, LC], F32, kind="ExternalInput")
    cvec = p.dram("cvec", [D, 2], F32, kind="ExternalInput")
    modw = p.dram("modw", [D, 2048], F32, kind="ExternalInput")
    modbT = p.dram("modbT", [128, 16], F32, kind="ExternalInput")
    w = p.dram("w", [D, 384], F32, kind="ExternalInput")
    cw = p.dram("cw", [128, 9], F32, kind="ExternalInput")
    cb = p.dram("cb", [128, 3], F32, kind="ExternalInput")
    skp = p.dram("skp", [128, 2], F32, kind="ExternalInput")
    featsT = p.dram("featsT", [33, L], F32, kind="ExternalInput")
    featsTc = p.dram("featsTc", [33, LC], F32, kind="ExternalInput")
    w1 = p.dram("w1", [33, 64], F32, kind="ExternalInput")
    w23 = p.dram("w23", [64, 128], F32, kind="ExternalInput")
    w4 = p.dram("w4", [64, 512], F32, kind="ExternalInput")
    fb = p.dram("fb", [64, 6], F32, kind="ExternalInput")
    tl = p.dram("tl", [1, L], F32, kind="ExternalInput")
    tlc = p.dram("tlc", [1, LC], F32, kind="ExternalInput")
    dl = p.dram("dl", [128, 1], F32, kind="ExternalInput")
    yhy = p.dram("yhy", [128, L + LC], F32, kind="ExternalOutput")
    xs = p.dram("xs", [2, 128, L + LC], F32, kind="Internal")

    banks = [p.psum(f"bank{j}", [128, 512], F32) for j in range(6)]
    fmv = compute_fm_mod(p, modw, modbT, cvec, banks)
    bZ = banks[0:3]
    bM, bF = banks[3], banks[4]

    w_s = p.sbuf("w_s", [128, 8, 384], BF16)
    p.dma("pool", w_s[:], w[:].rearrange("(k p) n -> p k n", p=128), [w], [w_s])
    cw_s = p.sbuf("cw_s", [128, 9], F32)
    p.dma("sp", cw_s[:], cw[:], [cw], [cw_s])
    cb_s = p.sbuf("cb_s", [128, 3], F32)
    p.dma("sp", cb_s[:], cb[:], [cb], [cb_s])
    sk_s = p.sbuf("sk_s", [128, 2], F32)
    p.dma("sp", sk_s[:], skp[:], [skp], [sk_s])
    dl_s = p.sbuf("dl_s", [128, 1], F32)
    p.dma("sp", dl_s[:], dl[:], [dl], [dl_s])
    w1_s = p.sbuf("w1_s", [33, 64], F32)
    p.dma("sp", w1_s[:], w1[:], [w1], [w1_s])
    w23_s = p.sbuf("w23_s", [64, 128], F32)
    p.dma("sp", w23_s[:], w23[:], [w23], [w23_s])
    w4_s = p.sbuf("w4_s", [64, 512], F32)
    p.dma("sp", w4_s[:], w4[:], [w4], [w4_s])
    fb_s = p.sbuf("fb_s", [64, 9], F32)
    p.dma("sp", fb_s[:, 0:6], fb[:], [fb], [fb_s])
    p.op("dve", lambda e: e.tensor_tensor(out=fb_s[:, 6:9], in0=fb_s[:, 0:3], in1=fb_s[:, 3:6], op=ALU.mult), [fb_s], [fb_s])
    cst = p.sbuf("cst", [64, 2], F32)
    p.op("dve", lambda e: e.memset(cst[:, 0:1], 1.0 / TWO_PI), [], [cst])
    p.op("dve", lambda e: e.memset(cst[:, 1:2], MAGIC), [cst], [cst])

    vbuf = {L: p.sbuf("vL", [128, L], F32), LC: p.sbuf("vC", [128, LC], F32)}

    p.begin_tmp()
    xb = p.sbuf("xb", [128, 8, 512], F32)
    hT = p.sbuf("hT", [128, 8, 512], BF16)
    ut = [p.sbuf(f"ut{j}", [128, 2048], F32) for j in range(2)]
    zf = {L: [p.sbuf(f"zf{s}", [128, L + 2], F32) for s in range(3)], LC: [p.sbuf(f"zfc{s}", [128, LC + 2], F32) for s in range(3)]}
    ucount = 0
    for (src, Lx, vv, roff) in ((cxT, LC, 1, L), (xT, L, 0, 0)):
        z3 = zf[Lx]
        for s in range(3):
            p.op("pool", lambda e: e.memset(z3[s][:, 0:1], 0.0), [], [z3[s]])
            p.op("pool", lambda e: e.memset(z3[s][:, Lx + 1:Lx + 2], 0.0), [z3[s]], [z3[s]])
        bl = min(512, Lx)
        for t0 in range(0, Lx, bl):
            p.dma("sp", xb[:, :, 0:bl], src[:, t0:t0 + bl].rearrange("(k p) n -> p k n", p=128), [src], [xb])
            for k in range(8):
                p.op("act", lambda e: e.activation(out=hT[:, k, 0:bl], in_=xb[:, k, 0:bl], func=AF.Identity,
                                                   scale=fmv[vv][:, 8 + k:9 + k], bias=fmv[vv][:, k:k + 1]), [xb, fmv[vv]], [hT])
            for s in range(3):
                p.mm(bZ[s], [(bZ[s][:, 0:bl], w_s[:, k, s * 128:(s + 1) * 128], hT[:, k, 0:bl]) for k in range(8)], [w_s, hT])
                p.op("act", lambda e: e.activation(out=z3[s][:, 1 + t0:1 + t0 + bl], in_=bZ[s][:, 0:bl], func=AF.Identity), [bZ[s]], [z3[s]])
        ch = min(2048, Lx)
        for s in range(3):
            for c0 in range(0, Lx, ch):
                if s == 2:
                    dstb, dst = vbuf[Lx], vbuf[Lx][:, c0:c0 + ch]
                else:
                    dstb = ut[ucount % 2]
                    ucount += 1
                    dst = dstb[:, 0:ch]
                p.op("act", lambda e: e.activation(out=dst, in_=z3[s][:, 1 + c0:1 + c0 + ch], func=AF.Identity,
                                                   scale=cw_s[:, s * 3 + 1:s * 3 + 2], bias=cb_s[:, s:s + 1]), [z3[s], cw_s, cb_s], [dstb])
                p.op("dve", lambda e: e.scalar_tensor_tensor(out=dst, in0=z3[s][:, c0:c0 + ch], scalar=cw_s[:, s * 3:s * 3 + 1], in1=dst,
                                                             op0=ALU.mult, op1=ALU.add), [z3[s], cw_s, dstb], [dstb])
                p.op("dve", lambda e: e.scalar_tensor_tensor(out=dst, in0=z3[s][:, 2 + c0:2 + c0 + ch], scalar=cw_s[:, s * 3 + 2:s * 3 + 3], in1=dst,
                                                             op0=ALU.mult, op1=ALU.add), [z3[s], cw_s, dstb], [dstb])
                if s < 2:
                    p.dma("sp", xs[s, :, roff + c0:roff + c0 + ch], dst, [dstb], [xs], group=True)
    p.end_tmp()

    a3 = {L: p.sbuf("a3L", [64, L], F32), LC: p.sbuf("a3C", [64, LC], F32)}
    filt = {L: p.sbuf("fL", [128, L], F32), LC: p.sbuf("fC", [128, LC], F32)}
    ybuf = {L: p.sbuf("yL", [128, L], F32), LC: p.sbuf("yC", [128, LC], F32)}
    f_s = p.sbuf("f_s", [33, 512], F32)
    t_y = p.sbuf("t_y", [64, 512], F32)
    t_k = p.sbuf("t_k", [64, 512], F32)
    t_a = [p.sbuf(f"t_a{j}", [64, 512], F32) for j in range(2)]
    tlb = p.sbuf("tlb", [128, 512], F32)
    wt = p.sbuf("wt", [128, 512], F32)
    sqt = p.sbuf("sqt", [128, 512], F32)
    ssq = p.sbuf("ssq", [128, 20], F32)
    xch = [p.sbuf(f"xch{j}", [128, 2048], F32) for j in range(2)]

    def sin_layer(src_ps, li, dst_ap, dst_buf, n):
        p.op("act", lambda e: e.activation(out=t_y[:, 0:n], in_=src_ps[0:64, 0:n], func=AF.Identity,
                                           scale=fb_s[:, li:li + 1], bias=fb_s[:, 6 + li:7 + li]), [src_ps, fb_s], [t_y])
        p.op("act", lambda e: e.activation(out=t_k[:, 0:n], in_=t_y[:, 0:n], func=AF.Identity,
                                           scale=cst[:, 0:1], bias=cst[:, 1:2]), [t_y, cst], [t_k])
        p.op("dve", lambda e: e.tensor_scalar(out=t_k[:, 0:n], in0=t_k[:, 0:n], scalar1=-MAGIC, scalar2=None, op0=ALU.add), [t_k], [t_k])
        p.op("dve", lambda e: e.scalar_tensor_tensor(out=t_y[:, 0:n], in0=t_k[:, 0:n], scalar=-TWO_PI, in1=t_y[:, 0:n],
                                                     op0=ALU.mult, op1=ALU.add), [t_k, t_y], [t_y])
        p.op("act", lambda e: e.activation(out=dst_ap, in_=t_y[:, 0:n], func=AF.Sin), [t_y], [dst_buf])

    xcount = 0
    for (Lx, fsrc, tsrc, roff) in ((LC, featsTc, tlc, L), (L, featsT, tl, 0)):
        bl = min(512, Lx)
        nblk = Lx // bl
        for bi in range(nblk):
            t0 = bi * bl
            p.dma("sp", f_s[:, 0:bl], fsrc[:, t0:t0 + bl], [fsrc], [f_s])
            p.mm(bM, [(bM[0:64, 0:bl], w1_s[:, :], f_s[:, 0:bl])], [w1_s, f_s])
            sin_layer(bM, 0, t_a[0][:, 0:bl], t_a[0], bl)
            p.mm(bM, [(bM[0:64, 0:bl], w23_s[:, 0:64], t_a[0][:, 0:bl])], [w23_s, t_a[0]])
            sin_layer(bM, 1, t_a[1][:, 0:bl], t_a[1], bl)
            p.mm(bM, [(bM[0:64, 0:bl], w23_s[:, 64:128], t_a[1][:, 0:bl])], [w23_s, t_a[1]])
            sin_layer(bM, 2, a3[Lx][:, t0:t0 + bl], a3[Lx], bl)
        ch = min(2048, Lx)
        v_, y_, fl = vbuf[Lx], ybuf[Lx], filt[Lx]
        for order in range(2):
            u_, acc = (v_, y_) if order == 0 else (y_, v_)
            for dr in range(2):
                col0 = (order * 2 + dr) * 128
                for bi in range(nblk):
                    t0 = bi * bl
                    p.mm(bF, [(bF[:, 0:bl], w4_s[:, col0:col0 + 128], a3[Lx][:, t0:t0 + bl])], [w4_s, a3[Lx]])
                    p.dma("sp", tlb[:, 0:bl], tsrc[0:1, t0:t0 + bl].partition_broadcast(128), [tsrc], [tlb])
                    p.op("act", lambda e: e.activation(out=wt[:, 0:bl], in_=tlb[:, 0:bl], func=AF.Exp, scale=dl_s[:, 0:1]), [tlb, dl_s], [wt])
                    p.op("dve", lambda e: e.tensor_tensor(out=fl[:, t0:t0 + bl], in0=bF[:, 0:bl], in1=wt[:, 0:bl], op=ALU.mult), [bF, wt], [fl])
                    p.op("act", lambda e: e.activation(out=sqt[:, 0:bl], in_=fl[:, t0:t0 + bl], func=AF.Square, accum_out=ssq[:, bi:bi + 1]),
                         [fl], [sqt, ssq])
                p.op("dve", lambda e: e.tensor_reduce(out=ssq[:, 16:17], in_=ssq[:, 0:nblk], axis=AX.X, op=ALU.add), [ssq], [ssq])
                p.op("act", lambda e: e.activation(out=ssq[:, 17:18], in_=ssq[:, 16:17], func=AF.Sqrt), [ssq], [ssq])
                p.op("dve", lambda e: e.reciprocal(out=ssq[:, 18:19], in_=ssq[:, 17:18]), [ssq], [ssq])
                for c0 in range(0, Lx, ch):
                    p.op("dve", lambda e: e.tensor_scalar(out=fl[:, c0:c0 + ch], in0=fl[:, c0:c0 + ch], scalar1=ssq[:, 18:19], scalar2=None,
                                                          op0=ALU.mult), [fl, ssq], [fl])
                for d in range(Lx):
                    n = Lx - d
                    if dr == 0:
                        o_ap, i_ap = acc[:, d:Lx], u_[:, 0:n]
                    else:
                        o_ap, i_ap = acc[:, 0:n], u_[:, d:Lx]
                    if dr == 0 and d == 0:
                        p.op("dve", lambda e: e.tensor_scalar(out=o_ap, in0=i_ap, scalar1=fl[:, 0:1], scalar2=None, op0=ALU.mult), [u_, fl], [acc])
                    else:
                        p.op("dve", lambda e: e.scalar_tensor_tensor(out=o_ap, in0=i_ap, scalar=fl[:, d:d + 1], in1=o_ap, op0=ALU.mult, op1=ALU.add),
                             [u_, fl, acc], [acc])
            for c0 in range(0, Lx, ch):
                xc = xch[xcount % 2]
                xcount += 1
                p.dma("sp", xc[:, 0:ch], xs[order, :, roff + c0:roff + c0 + ch], [xs], [xc])
                p.op("dve", lambda e: e.scalar_tensor_tensor(out=acc[:, c0:c0 + ch], in0=u_[:, c0:c0 + ch], scalar=sk_s[:, order:order + 1],
                                                             in1=acc[:, c0:c0 + ch], op0=ALU.mult, op1=ALU.add), [u_, sk_s, acc], [acc])
                p.op("dve", lambda e: e.tensor_tensor(out=acc[:, c0:c0 + ch], in0=acc[:, c0:c0 + ch], in1=xc[:, 0:ch], op=ALU.mult), [acc, xc], [acc])
                if order == 1:
                    p.dma("sp", yhy[:, roff + c0:roff + c0 + ch], acc[:, c0:c0 + ch], [acc], [yhy], group=True)
    p.finish([yhy])
    p.close()
    return nc

def _fm(v):
    return np.ascontiguousarray(v.reshape(8, 128).T)


def _run(nc, in_maps):
    return run_bass_kernel_spmd(nc, in_maps, core_ids=list(range(8))).results


def _common(I, layer, b):
    mb = I["mod_b"][layer]
    return dict(cvec=np.ascontiguousarray(np.stack([I["c"][b], I["c_ctx"]], 1)),
                modw=np.ascontiguousarray(I["mod_w"][layer][:, 0:2048]),
                modbT=np.concatenate([_fm(mb[0:1024]), _fm(mb[1024:2048])], 1))


def _ret_launch(I, xTs, cTs):
    nc = build_ret()
    idx = np.arange(128, dtype=np.float32)
    J, Ic = idx[:, None], idx[None, :]
    E = np.zeros((2, 128, 258), np.float32)
    E[0, :, 0:128] = np.maximum(Ic - J, 0); E[0, :, 128:256] = (idx + 1)[None, :]; E[0, :, 256] = 127 - idx; E[0, :, 257] = 128
    E[1, :, 0:128] = np.maximum(J - Ic, 0); E[1, :, 128:256] = (128 - idx)[None, :]; E[1, :, 256] = idx; E[1, :, 257] = 128
    msk = np.stack([(J <= Ic), (J >= Ic)]).astype(np.float32)
    t_ = np.arange(8192)
    inv = (10000.0 ** (-np.arange(32, dtype=np.float32) / 32)).astype(np.float32)
    ang = np.concatenate([(t_ // 64)[:, None] * inv, (t_ % 64)[:, None] * inv], -1).astype(np.float32)
    rope = np.concatenate([np.cos(ang), np.sin(ang)], -1).astype(np.float32)
    ropeT = np.ascontiguousarray(np.stack([np.concatenate([np.cos(ang), np.cos(ang)], -1).T,
                                           np.concatenate([-np.sin(ang), np.sin(ang)], -1).T])).astype(np.float32)
    W = I["ab_w_in"][0]
    maps = []
    for r in range(8):
        b, h = r // 4, r % 4
        w = np.concatenate([W[:, j * 512 + h * 128: j * 512 + (h + 1) * 128] for j in range(4)]
                           + [W[:, h * 128 + 64:h * 128 + 128], W[:, h * 128:h * 128 + 64],
                              W[:, 512 + h * 128 + 64:512 + h * 128 + 128], W[:, 512 + h * 128:512 + h * 128 + 64]], 1)
        lgs = np.array([[I["ret_log_decay_f"][0][h], I["ret_log_decay_b"][0][h]]], np.float32)
        m = dict(xT=xTs[b], cxT=cTs[b], w=np.ascontiguousarray(w), lgs=lgs, ecst=E, msk=msk, rope=rope, ropeT=ropeT,
                 identb=np.eye(128, dtype=np.float32))
        m.update(_common(I, 0, b))
        maps.append(m)
    return _run(nc, maps)


def _hy_launch(I, xTs, cTs):
    nc = build_hy()

    def feats(length):
        t = np.linspace(0.0, 1.0, length, dtype=np.float32)[:, None]
        w = (2.0 * math.pi * np.arange(length, dtype=np.float32)[:, None] / length).astype(np.float32)
        f = np.linspace(1e-4, 15, 16, dtype=np.float32)[None, :]
        return np.concatenate([t, np.cos(f * w), -np.sin(f * w)], -1).astype(np.float32), t[:, 0]
    fL, tL = feats(8192)
    fC, tC = feats(256)
    deltas = np.abs(np.linspace(math.log(1e-2) / 1.5, math.log(1e-2) / 0.3, 512, dtype=np.float32))
    W = I["ab_w_in"][0]; CW = I["hy_conv_w"][0]; CB = I["hy_conv_b"][0]
    fb = np.concatenate([I["hy_freq"][0].T, np.stack([I["hy_b1"][0], I["hy_b2"][0], I["hy_b3"][0]], 1)], 1).astype(np.float32)
    maps = []
    for r in range(8):
        b, g = r // 4, r % 4
        cs = slice(g * 128, (g + 1) * 128)
        w = np.concatenate([W[:, 2048 + s * 512 + g * 128: 2048 + s * 512 + (g + 1) * 128] for s in range(3)], 1)
        cw = np.stack([CW[tap, s * 512 + g * 128: s * 512 + (g + 1) * 128] for s in range(3) for tap in range(3)], 1)
        cb = np.stack([CB[s * 512 + g * 128: s * 512 + (g + 1) * 128] for s in range(3)], 1)
        w4 = np.concatenate([I["hy_w4"][0][:, o * 1024 + d * 512 + g * 128: o * 1024 + d * 512 + (g + 1) * 128]
                             for o in range(2) for d in range(2)], 1)
        m = dict(xT=xTs[b], cxT=cTs[b], w=np.ascontiguousarray(w), cw=np.ascontiguousarray(cw), cb=np.ascontiguousarray(cb),
                 skp=np.ascontiguousarray(I["hy_skip"][0][:, cs].T), featsT=np.ascontiguousarray(fL.T),
                 featsTc=np.ascontiguousarray(fC.T), w1=I["hy_w1"][0],
                 w23=np.ascontiguousarray(np.concatenate([I["hy_w2"][0], I["hy_w3"][0]], 1)), w4=np.ascontiguousarray(w4),
                 fb=np.ascontiguousarray(fb), tl=tL[None, :].copy(), tlc=tC[None, :].copy(),
                 dl=np.ascontiguousarray(-deltas[cs][:, None]))
        m.update(_common(I, 0, b))
        maps.append(m)
    return _run(nc, maps)


def _ffn_launch(layer, feat, featc, xres, cres, I, w_out):
    has_ctx = featc is not None
    nc = build_ffn(has_ctx, kin=feat.shape[-1])
    mb = I["mod_b"][layer]
    modbT = np.concatenate([_fm(mb[3072:4096]), _fm(mb[4096:5120])], 1)
    ident = np.eye(128, dtype=np.float32)
    maps = []
    for r in range(8):
        b, q = r // 4, r % 4
        ts = slice(q * 2048, (q + 1) * 2048)
        cs = slice(q * 64, (q + 1) * 64)
        y_ = feat[b, ts] if not has_ctx else np.concatenate([feat[b, ts], featc[b, cs]], 0)
        x_ = xres[b, ts] if not has_ctx else np.concatenate([xres[b, ts], cres[b, cs]], 0)
        maps.append(dict(yT=np.ascontiguousarray(y_.T), xres=np.ascontiguousarray(x_),
                         cvec=np.ascontiguousarray(np.stack([I["c"][b], I["c_ctx"]], 1)),
                         modw=np.ascontiguousarray(I["mod_w"][layer][:, 2048:]), modb=np.ascontiguousarray(mb[None, 2048:]),
                         modbT=modbT, lng=np.ascontiguousarray(I["ln_g"][layer]), lnb=np.ascontiguousarray(I["ln_b"][layer]),
                         wout=np.ascontiguousarray(w_out), rw=I["router_w"], rbias=np.ascontiguousarray(I["router_bias"][None, :]),
                         wg=np.ascontiguousarray(I["exp_w_gate"][layer]), wu=np.ascontiguousarray(I["exp_w_up"][layer]),
                         wd=np.ascontiguousarray(I["exp_w_down"][layer]), ident=ident))
    res = _run(nc, maps)
    x_out = np.zeros((2, 8192, 1024), np.float32)
    c_out = np.zeros((2, 256, 1024), np.float32) if has_ctx else None
    for r in range(8):
        b, q = r // 4, r % 4
        o = res[r]["out"]
        x_out[b, q * 2048:(q + 1) * 2048] = o[:2048]
        if has_ctx:
            c_out[b, q * 64:(q + 1) * 64] = o[2048:]
    return x_out, c_out


def _gla_launch(x1, c1, I):
    layer = 1
    nc = build_gla()
    idx = np.arange(128)
    same = (idx[:, None] // 64) == (idx[None, :] // 64)
    tri_f = (same & (idx[:, None] <= idx[None, :])).astype(np.float32)
    tri_b = (same & (idx[:, None] >= idx[None, :])).astype(np.float32)
    U_f = (same & (idx[:, None] > idx[None, :])).astype(np.float32)
    U_b = (same & (idx[:, None] < idx[None, :])).astype(np.float32)
    cst = (np.stack([tri_f, tri_b, U_f, U_b]) / 16.0).astype(np.float32)
    msk = np.stack([tri_f, tri_b])
    W = I["gla_w_in"][0]
    maps = []
    for r in range(8):
        b, h = r // 4, r % 4
        w = np.concatenate([W[:, h * 128:(h + 1) * 128], W[:, 512 + h * 128:512 + (h + 1) * 128],
                            W[:, 1024 + h * 256:1024 + (h + 1) * 256], W[:, 2048 + h * 256:2048 + (h + 1) * 256], W[:, 3072:3104]], 1)
        gwe = np.stack([np.concatenate([I["gla_gate_w_f"][0][:, h * 128:(h + 1) * 128], I["gla_gate_b_f"][0][None, h * 128:(h + 1) * 128]], 0),
                        np.concatenate([I["gla_gate_w_b"][0][:, h * 128:(h + 1) * 128], I["gla_gate_b_b"][0][None, h * 128:(h + 1) * 128]], 0)])
        m = dict(xT=np.ascontiguousarray(x1[b].T), cxT=np.ascontiguousarray(c1[b].T), w=np.ascontiguousarray(w),
                 gwe=np.ascontiguousarray(gwe), cst=cst, msk=msk)
        m.update(_common(I, layer, b))
        maps.append(m)
    res = _run(nc, maps)
    feat = np.zeros((2, 8192, 1024), np.float32)
    for r in range(8):
        b, h = r // 4, r % 4
        feat[b, :, h * 256:(h + 1) * 256] = res[r]["feat"]
    return feat


def kernel(**inputs):
    I = {k: np.ascontiguousarray(np.asarray(v, dtype=np.float32)) for k, v in inputs.items()}
    x, ctx = I["x"], I["ctx"]
    xTs = [np.ascontiguousarray(x[b].T) for b in range(2)]
    cTs = [np.ascontiguousarray(ctx[b].T) for b in range(2)]
    feat0 = np.zeros((2, 8192, 1024), np.float32)
    featc0 = np.zeros((2, 256, 1024), np.float32)
    res = _ret_launch(I, xTs, cTs)
    for r in range(8):
        b, h = r // 4, r % 4
        y = res[r]["yret"]
        feat0[b, :, h * 128:(h + 1) * 128] = y[:8192]
        featc0[b, :, h * 128:(h + 1) * 128] = y[8192:]
    res = _hy_launch(I, xTs, cTs)
    for r in range(8):
        b, g = r // 4, r % 4
        y = res[r]["yhy"]
        feat0[b, :, 512 + g * 128:512 + (g + 1) * 128] = y[:, :8192].T
        featc0[b, :, 512 + g * 128:512 + (g + 1) * 128] = y[:, 8192:].T
    x1, c1 = _ffn_launch(0, feat0, featc0, x, ctx, I, I["ab_w_out"][0])
    feat1 = _gla_launch(x1, c1, I)
    out, _ = _ffn_launch(1, feat1, None, x1, None, I, I["gla_w_out"][0])
    return out.astype(np.float32)
```
